# Optimizing a Trainium2 kernel written in Bass

```python
import math
import jax, jax.numpy as jnp
from jax import lax
import numpy as np

D_MODEL = 1024
BATCH = 8
SEQ = 4096
DEPTH = 2

EPS = 1e-6
BLOCK_Q = 128
D_FF = 2816
GLA_HEADS = 4
GLA_DK = 64
GLA_DV = 128
GLA_GATE_RANK = 16
GLA_GATE_NORMALIZER = 16.0
GLA_CHUNK = 64
FOX_HEADS = 8
FOX_DH = 64
DSA_HEADS = 16
DSA_DH = 64
DSA_LATENT = 256
IDX_HEADS = 8
IDX_DIM = 64
TOPK_MAX = 256
T5_BUCKETS = 32
T5_MAX_DIST = 128

N_EVEN = (DEPTH + 1) // 2
N_ODD = DEPTH // 2

EVEN_SIZES = (GLA_HEADS * GLA_DK, GLA_HEADS * GLA_DK, GLA_HEADS * GLA_DV, GLA_HEADS * GLA_DV, GLA_GATE_RANK,
              FOX_HEADS * FOX_DH, FOX_HEADS * FOX_DH, FOX_HEADS * FOX_DH, FOX_HEADS)
ODD_SIZES = (DSA_HEADS * DSA_DH, DSA_LATENT, IDX_HEADS * IDX_DIM, IDX_DIM, IDX_HEADS)
D_IN_EVEN = sum(EVEN_SIZES)
D_IN_ODD = sum(ODD_SIZES)
EVEN_SPLITS = tuple(int(v) for v in np.cumsum(EVEN_SIZES)[:-1])
ODD_SPLITS = tuple(int(v) for v in np.cumsum(ODD_SIZES)[:-1])
D_MIX_EVEN = GLA_HEADS * GLA_DV + FOX_HEADS * FOX_DH
D_MIX_ODD = DSA_HEADS * DSA_DH

kernel_name = "hybrid_gla_fox_dsa_macaron"


def rmsnorm(x, g):
    xf = x.astype(jnp.float32)
    y = xf * lax.rsqrt(jnp.mean(xf * xf, axis=-1, keepdims=True) + EPS)
    return (y * g.astype(jnp.float32)).astype(x.dtype)


def swiglu(h, w_in, w_out):
    a, b = jnp.split(h @ w_in, 2, axis=-1)
    return (jax.nn.silu(a) * b) @ w_out


def to_blocks(t, nb):
    return t.reshape((t.shape[0], nb, BLOCK_Q) + t.shape[2:]).swapaxes(0, 1)


def gla_chunked(q, k, v, gk):
    B, S, H, dk = q.shape
    dv = v.shape[-1]
    C = GLA_CHUNK
    N = S // C

    def chunks(t):
        return t.astype(jnp.float32).reshape(B, N, C, H, t.shape[-1]).transpose(1, 0, 3, 2, 4)

    qc = chunks(q) * (dk ** -0.5)
    kc, vc, gc = chunks(k), chunks(v), chunks(gk)
    G = jnp.cumsum(gc, axis=-2)
    G_last = G[..., -1, :]
    q_dec = qc * jnp.exp(G)
    k_dec = kc * jnp.exp(-G)
    k_to_end = kc * jnp.exp(G_last[..., None, :] - G)
    causal = jnp.tril(jnp.ones((C, C), dtype=bool))
    A = jnp.where(causal, jnp.einsum('nbhid,nbhjd->nbhij', q_dec, k_dec), 0.0)
    o_intra = jnp.einsum('nbhij,nbhjv->nbhiv', A, vc)

    def step(state, inp):
        qd, ke, vv, gl = inp
        o = jnp.einsum('bhid,bhdv->bhiv', qd, state)
        state = jnp.exp(gl)[..., None] * state + jnp.einsum('bhjd,bhjv->bhdv', ke, vv)
        return state, o

    state0 = jnp.zeros((B, H, dk, dv), jnp.float32)
    _, o_inter = lax.scan(step, state0, (q_dec, k_to_end, vc, G_last))
    o = o_intra + o_inter
    return o.transpose(1, 0, 3, 2, 4).reshape(B, S, H, dv)


def fox_attention(q, k, v, log_f):
    B, S, H, dh = q.shape
    nb = S // BLOCK_Q
    F = jnp.cumsum(log_f, axis=1)
    F_keys = F.transpose(0, 2, 1)
    key_pos = jnp.arange(S)

    def one_block(args):
        i, qb, Fb = args
        q_pos = i * BLOCK_Q + jnp.arange(BLOCK_Q)
        s = jnp.einsum('bqhd,bkhd->bhqk', qb, k).astype(jnp.float32) * (dh ** -0.5)
        s = s + (Fb.transpose(0, 2, 1)[..., :, None] - F_keys[..., None, :])
        s = jnp.where(key_pos[None, :] <= q_pos[:, None], s, -jnp.inf)
        p = jax.nn.softmax(s, axis=-1)
        return jnp.einsum('bhqk,bkhd->bqhd', p.astype(v.dtype), v)

    o = lax.map(one_block, (jnp.arange(nb), to_blocks(q, nb), to_blocks(F, nb)))
    return o.swapaxes(0, 1).reshape(B, S, H, dh)


def t5_bucket(dist):
    max_exact = T5_BUCKETS // 2
    d = jnp.maximum(dist, 1).astype(jnp.float32)
    large = max_exact + (jnp.log(d / max_exact) / math.log(T5_MAX_DIST / max_exact)
                         * (T5_BUCKETS - max_exact)).astype(jnp.int32)
    large = jnp.minimum(large, T5_BUCKETS - 1)
    return jnp.where(dist < max_exact, dist, large)


def dsa_attention(q_lat, c, q_idx, k_idx, w_idx, t5_table):
    B, S, H, DL = q_lat.shape
    k_top = min(TOPK_MAX, S // 4)
    nb = S // BLOCK_Q
    key_pos = jnp.arange(S)
    w_scaled = w_idx.astype(jnp.float32) * (IDX_HEADS ** -0.5)
    gather = jax.vmap(lambda cb, ib: cb[ib])

    def one_block(args):
        i, qb, qib, wb = args
        q_pos = i * BLOCK_Q + jnp.arange(BLOCK_Q)
        logit_idx = jnp.einsum('bqhd,bkd->bqhk', qib, k_idx).astype(jnp.float32) * (IDX_DIM ** -0.5)
        score = jnp.einsum('bqh,bqhk->bqk', wb, jax.nn.relu(logit_idx))
        score = jnp.where((key_pos[None, :] <= q_pos[:, None])[None], score, -jnp.inf)
        _, idx = lax.top_k(score, k_top)
        c_sel = gather(c, idx)
        dist = q_pos[None, :, None] - idx
        bias = t5_table[t5_bucket(jnp.maximum(dist, 0))]
        s = jnp.einsum('bqhl,bqkl->bqhk', qb, c_sel).astype(jnp.float32) * (DSA_DH ** -0.5)
        s = s + bias.transpose(0, 1, 3, 2).astype(jnp.float32)
        s = jnp.where((dist >= 0)[:, :, None, :], s, -jnp.inf)
        p = jax.nn.softmax(s, axis=-1)
        return jnp.einsum('bqhk,bqkl->bqhl', p.astype(c.dtype), c_sel)

    o = lax.map(one_block, (jnp.arange(nb), to_blocks(q_lat, nb), to_blocks(q_idx, nb), to_blocks(w_scaled, nb)))
    return o.swapaxes(0, 1).reshape(B, S, H, DL)


def even_mixer(h, w_in, gla_w_gate, gla_b_gate, gla_norm_g, fox_b_f, w_out):
    B, S, _ = h.shape
    q_g, k_g, v_g, g_out, gk_low, q_f, k_f, v_f, f_logit = jnp.split(h @ w_in, EVEN_SPLITS, axis=-1)
    gk = jax.nn.log_sigmoid((gk_low @ gla_w_gate + gla_b_gate).astype(jnp.float32)) / GLA_GATE_NORMALIZER
    o_gla = gla_chunked(q_g.reshape(B, S, GLA_HEADS, GLA_DK), k_g.reshape(B, S, GLA_HEADS, GLA_DK),
                        v_g.reshape(B, S, GLA_HEADS, GLA_DV), gk.reshape(B, S, GLA_HEADS, GLA_DK)).astype(h.dtype)
    o_gla = rmsnorm(o_gla, gla_norm_g) * jax.nn.silu(g_out.reshape(B, S, GLA_HEADS, GLA_DV))
    log_f = jax.nn.log_sigmoid((f_logit + fox_b_f).astype(jnp.float32))
    o_fox = fox_attention(q_f.reshape(B, S, FOX_HEADS, FOX_DH), k_f.reshape(B, S, FOX_HEADS, FOX_DH),
                          v_f.reshape(B, S, FOX_HEADS, FOX_DH), log_f)
    o = jnp.concatenate([o_gla.reshape(B, S, -1), o_fox.reshape(B, S, -1)], axis=-1)
    return o @ w_out


def odd_mixer(h, w_in, kv_norm_g, w_uk, w_uv, w_out, t5_table):
    B, S, _ = h.shape
    q, ckv, q_idx, k_idx, w_idx = jnp.split(h @ w_in, ODD_SPLITS, axis=-1)
    c = rmsnorm(ckv, kv_norm_g)
    q_lat = jnp.einsum('bshd,hdl->bshl', q.reshape(B, S, DSA_HEADS, DSA_DH), w_uk)
    o_lat = dsa_attention(q_lat, c, q_idx.reshape(B, S, IDX_HEADS, IDX_DIM), k_idx, w_idx, t5_table)
    o = jnp.einsum('bshl,hld->bshd', o_lat, w_uv).reshape(B, S, D_MIX_ODD)
    return o @ w_out


def setup_inputs(seed: int = 0) -> dict:
    key = jax.random.key(seed)
    ks = jax.random.split(key, 17)
    f32 = jnp.float32

    def nrm(k, shape, fan_in):
        return jax.random.normal(k, shape, f32) * (fan_in ** -0.5)

    def gain(k, shape):
        return 1.0 + 0.02 * jax.random.normal(k, shape, f32)

    return {
        "x": jax.random.normal(ks[0], (BATCH, SEQ, D_MODEL), f32),
        "norm_g": gain(ks[1], (DEPTH, 3, D_MODEL)),
        "ffn_w_in": nrm(ks[2], (DEPTH, 2, D_MODEL, 2 * D_FF), D_MODEL),
        "ffn_w_out": nrm(ks[3], (DEPTH, 2, D_FF, D_MODEL), D_FF),
        "even_w_in": nrm(ks[4], (N_EVEN, D_MODEL, D_IN_EVEN), D_MODEL),
        "gla_w_gate": nrm(ks[5], (N_EVEN, GLA_GATE_RANK, GLA_HEADS * GLA_DK), GLA_GATE_RANK),
        "gla_b_gate": 0.02 * jax.random.normal(ks[6], (N_EVEN, GLA_HEADS * GLA_DK), f32),
        "gla_norm_g": gain(ks[7], (N_EVEN, GLA_DV)),
        "fox_b_f": 2.0 + 0.5 * jax.random.normal(ks[8], (N_EVEN, FOX_HEADS), f32),
        "even_w_out": nrm(ks[9], (N_EVEN, D_MIX_EVEN, D_MODEL), D_MIX_EVEN),
        "odd_w_in": nrm(ks[10], (N_ODD, D_MODEL, D_IN_ODD), D_MODEL),
        "mla_kv_norm_g": gain(ks[11], (N_ODD, DSA_LATENT)),
        "mla_w_uk": nrm(ks[12], (N_ODD, DSA_HEADS, DSA_DH, DSA_LATENT), DSA_LATENT),
        "mla_w_uv": nrm(ks[13], (N_ODD, DSA_HEADS, DSA_LATENT, DSA_DH), DSA_LATENT),
        "odd_w_out": nrm(ks[14], (N_ODD, D_MIX_ODD, D_MODEL), D_MIX_ODD),
        "t5_table": 0.5 * jax.random.normal(ks[15], (T5_BUCKETS, DSA_HEADS), f32),
        "final_norm_g": gain(ks[16], (D_MODEL,)),
    }


def reference(x, norm_g, ffn_w_in, ffn_w_out, even_w_in, gla_w_gate, gla_b_gate, gla_norm_g, fox_b_f,
              even_w_out, odd_w_in, mla_kv_norm_g, mla_w_uk, mla_w_uv, odd_w_out, t5_table, final_norm_g):
    for layer in range(DEPTH):
        g = norm_g[layer]
        x = x + 0.5 * swiglu(rmsnorm(x, g[0]), ffn_w_in[layer, 0], ffn_w_out[layer, 0])
        h = rmsnorm(x, g[1])
        j = layer // 2
        if layer % 2 == 0:
            x = x + even_mixer(h, even_w_in[j], gla_w_gate[j], gla_b_gate[j], gla_norm_g[j], fox_b_f[j], even_w_out[j])
        else:
            x = x + odd_mixer(h, odd_w_in[j], mla_kv_norm_g[j], mla_w_uk[j], mla_w_uv[j], odd_w_out[j], t5_table)
        x = x + 0.5 * swiglu(rmsnorm(x, g[2]), ffn_w_in[layer, 1], ffn_w_out[layer, 1])
    return rmsnorm(x, final_norm_g)
```

```python
import copy
import os
import numpy as np

PARTS = os.environ.get("EVEN_PARTS", "gvshp")
GLA_STOP = float(os.environ.get("GLA_STOP", "99"))
OPARTS = os.environ.get("ODD_PARTS", "12abc4")
ODD_STOP = float(os.environ.get("ODD_STOP", "99"))
import concourse.bass as bass
import concourse.mybir as mybir
from concourse.bass_utils import run_bass_kernel_spmd

F32 = mybir.dt.float32
BF16 = mybir.dt.bfloat16
AF = mybir.ActivationFunctionType
ALU = mybir.AluOpType
AX = mybir.AxisListType

S = 4096
D = 1024
DFF = 2816
NT = S // 128
EPS = 1e-6
SB_BASE = 16384 + 128
ARENA_BYTES = 204 * 1024


class Buf:
    __slots__ = ("name", "w", "r", "dsem")

    def __init__(self, name=""):
        self.name = name
        self.w = {}
        self.r = {}
        self.dsem = None


class DSem:
    __slots__ = ("sem", "val", "key", "qt")

    def __init__(self, sem, key, qt):
        self.sem = sem
        self.val = 0
        self.key = key
        self.qt = qt


class Prog:
    ENGS = ("pe", "act", "dve", "pool", "sp")

    def __init__(self, nc, n_dsem=90):
        self.nc = nc
        self.lists = {e: [] for e in self.ENGS}
        self.cnt = {e: 0 for e in self.ENGS}
        self.seen = {e: {} for e in self.ENGS}
        self.sem = {e: nc.alloc_semaphore("s_" + e) for e in self.ENGS}
        self.dfree = {"hw": [DSem(nc.alloc_semaphore("d%d" % i), "d%d" % i, "hw") for i in range(n_dsem - 24)],
                      "sw": [DSem(nc.alloc_semaphore("w%d" % i), "w%d" % i, "sw") for i in range(24)]}
        self.dused = []
        self.bufs = []
        self.sb_off = SB_BASE
        self.uid = 0
        self.arena = nc.alloc_sbuf_tensor("arena", [128, ARENA_BYTES // 4], F32)

    def buf(self, name=""):
        b = Buf(name)
        self.bufs.append(b)
        return b

    def bufs_n(self, n, name=""):
        return [self.buf(name + str(i)) for i in range(n)]

    def sb(self, shape, dtype, name="t"):
        esz = 2 if dtype == BF16 else 4
        n = 1
        for s_ in shape[1:]:
            n *= s_
        nwords = ((n * esz + 63) // 64 * 64) // 4
        w0 = (self.sb_off - SB_BASE) // 4
        self.sb_off += nwords * 4
        assert self.sb_off <= SB_BASE + ARENA_BYTES, ("SBUF overflow", self.sb_off)
        v = self.arena[0:shape[0], w0:w0 + nwords]
        if dtype == BF16:
            v = v.bitcast(BF16)
        v = v[:, 0:n]
        if len(shape) == 3:
            v = v.rearrange("p (a b) -> p a b", a=shape[1])
        elif len(shape) == 4:
            v = v.rearrange("p (a b c) -> p a b c", a=shape[1], b=shape[2])
        return v

    def _deps(self, reads, writes):
        deps = {}
        for b in reads:
            for k, v in b.w.items():
                if k not in deps or deps[k][1] < v[1]:
                    deps[k] = v
        for b in writes:
            for dd in (b.w, b.r):
                for k, v in dd.items():
                    if k not in deps or deps[k][1] < v[1]:
                        deps[k] = v
        return deps

    def _filter(self, eng, deps):
        waits = []
        for k, (s_, c) in deps.items():
            if k == eng:
                if eng == "pe":
                    continue
                if c <= self.cnt[eng] - 3:
                    continue
            if self.seen[eng].get(k, 0) >= c:
                continue
            self.seen[eng][k] = c
            waits.append((s_, c))
        return waits

    def op(self, eng, fn, reads=(), writes=()):
        waits = self._filter(eng, self._deps(reads, writes))
        self.cnt[eng] += 1
        tok = (self.sem[eng], self.cnt[eng])
        self.lists[eng].append(("op", waits, fn))
        for b in reads:
            b.r[eng] = tok
        for b in writes:
            b.w[eng] = tok
        return tok

    def dma(self, q, out_ap, in_ap, reads=(), writes=(), **kw):
        waits = self._filter(q, self._deps(reads, writes))
        b0 = writes[0]
        qt = "sw" if q == "pool" else "hw"
        if b0.dsem is None:
            b0.dsem = self.dfree[qt].pop()
            self.dused.append(b0.dsem)
        ds = b0.dsem
        assert ds.qt == qt, ("buffer DMA-written from both SW and HW DGE", b0.name)
        ds.val += 16
        tok = (ds.sem, ds.val)
        self.lists[q].append(("dma", waits, out_ap, in_ap, ds.sem, kw))
        for b in reads:
            b.r[ds.key] = tok
        for b in writes:
            b.w[ds.key] = tok
        return tok

    def barrier(self, keep=False):
        for e in self.ENGS:
            waits = []
            for e2 in self.ENGS:
                if e2 != e and self.cnt[e2] > self.seen[e].get(e2, 0):
                    waits.append((self.sem[e2], self.cnt[e2]))
                    self.seen[e][e2] = self.cnt[e2]
            for ds in self.dused:
                waits.append((ds.sem, ds.val))
            self.lists[e].append(("wait", waits))
        if keep:
            return
        for ds in self.dused:
            self.dfree[ds.qt].append(ds)
        self.dused = []
        for b in self.bufs:
            b.w = {}
            b.r = {}
            b.dsem = None
        self.sb_off = SB_BASE

    def emit(self):
        nc = self.nc
        with nc.Block() as block:
            def mk(name):
                def body(e):
                    own = self.sem[name]
                    for item in self.lists[name]:
                        for (s_, c) in item[1]:
                            e.wait_ge(s_, c)
                        if item[0] == "op":
                            item[2](e).then_inc(own, 1)
                        elif item[0] == "dma":
                            e.dma_start(out=item[2], in_=item[3], **item[5]).then_inc(item[4], 16)
                return body
            block.tensor(mk("pe"))
            block.scalar(mk("act"))
            block.vector(mk("dve"))
            block.gpsimd(mk("pool"))
            block.sync(mk("sp"))


class Ctx:
    pass


def load_norm_transpose(P, C, src, srcbuf, gB, gBb, xt, xtb, hT, hTb, tiles, keep_x=True):
    nc = P.nc
    n = len(tiles)
    for i, tt in enumerate(tiles):
        P.dma("sp", xt[:, i, :], src[tt * 128:(tt + 1) * 128, :], reads=[srcbuf], writes=[xtb[i]])
    for i, tt in enumerate(tiles):
        k = i % 2
        junk, junkb = C.junk[k], C.junkb[k]
        ss, ssb = C.ss, C.ssb[k]
        P.op("act", lambda e, i=i, k=k: e.activation(out=C.junk[k][:, :], in_=xt[:, i, :], func=AF.Square,
                                                     accum_out=C.ss[:, k:k + 1]),
             reads=[xtb[i]], writes=[junkb, ssb])
        P.op("dve", lambda e, k=k: e.tensor_scalar(out=C.ss2[:, k:k + 1], in0=C.ss[:, k:k + 1], scalar1=1.0 / D,
                                                   scalar2=EPS, op0=ALU.mult, op1=ALU.add),
             reads=[ssb], writes=[C.ss2b[k]])
        P.op("act", lambda e, k=k: e.activation(out=C.ss3[:, k:k + 1], in_=C.ss2[:, k:k + 1], func=AF.Sqrt),
             reads=[C.ss2b[k]], writes=[C.ss3b[k]])
        P.op("dve", lambda e, k=k: e.reciprocal(out=C.rstd[:, k:k + 1], in_=C.ss3[:, k:k + 1]),
             reads=[C.ss3b[k]], writes=[C.rstdb[k]])
        P.op("dve", lambda e, i=i, k=k: e.scalar_tensor_tensor(out=C.hn[k][:, :], in0=xt[:, i, :],
                                                               scalar=C.rstd[:, k:k + 1], in1=gB[:, :],
                                                               op0=ALU.mult, op1=ALU.mult),
             reads=[xtb[i], C.rstdb[k], gBb], writes=[C.hnb[k]])
        for dc in range(8):
            P.op("pe", lambda e, dc=dc, k=k: e.transpose(out=C.psT[k][:, dc * 128:(dc + 1) * 128],
                                                         in_=C.hn[k][:, dc * 128:(dc + 1) * 128],
                                                         identity=C.ident[:, :]),
                 reads=[C.hnb[k], C.identb], writes=[C.psTb[k]])
        eng = "act" if (i % 2 == 0) else "dve"
        if eng == "act":
            P.op("act", lambda e, i=i, k=k: e.copy(out=hT[:, :, i * 128:(i + 1) * 128],
                                                   in_=C.psT[k][:, :].rearrange("p (c t) -> p c t", c=8)),
                 reads=[C.psTb[k]], writes=[hTb[i]])
        else:
            P.op("dve", lambda e, i=i, k=k: e.tensor_copy(out=hT[:, :, i * 128:(i + 1) * 128],
                                                          in_=C.psT[k][:, :].rearrange("p (c t) -> p c t", c=8)),
                 reads=[C.psTb[k]], writes=[hTb[i]])


def alloc_common(P, C):
    C.ident = P.sb([128, 128], BF16, "ident")
    C.identb = P.buf("ident")
    C.identf = P.sb([128, 128], F32, "identf")
    C.junk = [P.sb([128, 1024], BF16, "junk") for _ in range(2)]
    C.junkb = P.bufs_n(2, "junk")
    C.ss = P.sb([128, 2], F32, "ss")
    C.ss2 = P.sb([128, 2], F32, "ss2")
    C.ss3 = P.sb([128, 2], F32, "ss3")
    C.rstd = P.sb([128, 2], F32, "rstd")
    C.ssb = P.bufs_n(2, "ss")
    C.ss2b = P.bufs_n(2, "ss2")
    C.ss3b = P.bufs_n(2, "ss3")
    C.rstdb = P.bufs_n(2, "rstd")
    C.hn = [P.sb([128, 1024], BF16, "hn") for _ in range(2)]
    C.hnb = P.bufs_n(2, "hn")
    C.psTb = P.bufs_n(2, "psT")
    P.dma("sp", C.ident[:, :], C.ident_dram[:, :], reads=[], writes=[C.identb])
    P.dma("sp", C.identf[:, :], C.identf_dram[:, :], reads=[], writes=[C.identb])


def op_act(P, out, in_, func, reads, writes, **kw):
    return P.op("act", lambda e: e.activation(out=out, in_=in_, func=func, **kw), reads=reads, writes=writes)


def op_mm(P, out, lhsT, rhs, start, stop, reads, writes):
    return P.op("pe", lambda e: e.matmul(out, lhsT, rhs, start=start, stop=stop), reads=reads, writes=writes)


def op_tt(P, eng, out, in0, in1, op, reads, writes):
    return P.op(eng, lambda e: e.tensor_tensor(out=out, in0=in0, in1=in1, op=op), reads=reads, writes=writes)


def op_ts(P, eng, out, in0, s1, s2, op0, op1, reads, writes, **kw):
    if op1 is None:
        return P.op(eng, lambda e: e.tensor_scalar(out=out, in0=in0, scalar1=s1, scalar2=None, op0=op0, **kw),
                    reads=reads, writes=writes)
    return P.op(eng, lambda e: e.tensor_scalar(out=out, in0=in0, scalar1=s1, scalar2=s2, op0=op0, op1=op1, **kw),
                reads=reads, writes=writes)


def op_stt(P, out, in0, scalar, in1, op0, op1, reads, writes):
    return P.op("dve", lambda e: e.scalar_tensor_tensor(out=out, in0=in0, scalar=scalar, in1=in1, op0=op0, op1=op1),
                reads=reads, writes=writes)


def op_copy(P, eng, out, in_, reads, writes):
    if eng == "act":
        return P.op("act", lambda e: e.copy(out=out, in_=in_), reads=reads, writes=writes)
    return P.op(eng, lambda e: e.tensor_copy(out=out, in_=in_), reads=reads, writes=writes)


def ffn_phase(P, C, src, dst, xbuf, g_dram, w_in, w_out, final_g=None, out_dram=None):
    nc = P.nc
    alloc_common(P, C)
    C = copy.copy(C)
    TP = 1024
    NP = S // TP
    gB = P.sb([128, D], F32, "gB")
    gBb = P.buf("gB")
    P.dma("sp", gB[:, :], g_dram.partition_broadcast(128), reads=[], writes=[gBb])
    xt = P.sb([128, 8, D], F32, "xt")
    xtb = P.bufs_n(8, "xt")
    hT = P.sb([128, 8, TP], BF16, "hT")
    hTb = P.bufs_n(8, "hT")
    gT = P.sb([128, 22, TP], BF16, "gT")
    gTb = [[P.buf("gT") for _ in range(2)] for _ in range(22)]
    NWB = 3
    winA = [P.sb([128, 8, 256], BF16, "winA") for _ in range(NWB)]
    winB = [P.sb([128, 8, 256], BF16, "winB") for _ in range(NWB)]
    winAb = P.bufs_n(NWB, "winA")
    winBb = P.bufs_n(NWB, "winB")
    wout = P.sb([128, 22, D], BF16, "wout")
    woutb = P.bufs_n(2, "wout")
    sg = [P.sb([128, 512], F32, "sg") for _ in range(2)]
    sgb = P.bufs_n(2, "sg")
    w_out_v = w_out.rearrange("(fc p) n -> p fc n", p=128)
    for h in range(2):
        P.dma("pool", wout[:, h * 11:(h + 1) * 11, :], w_out_v[:, h * 11:(h + 1) * 11, :], reads=[], writes=[woutb[h]])
    w_in_v = w_in.rearrange("(dc p) n -> p dc n", p=128)
    blk = 0
    for p in range(NP):
        tiles = list(range(p * 8, p * 8 + 8))
        load_norm_transpose(P, C, src, xbuf, gB, gBb, xt, xtb, hT, hTb, tiles)
        for jb in range(11):
            wb = blk % NWB
            blk += 1
            P.dma("pool", winA[wb][:, :, :], w_in_v[:, :, jb * 256:(jb + 1) * 256], reads=[], writes=[winAb[wb]])
            P.dma("pool", winB[wb][:, :, :], w_in_v[:, :, DFF + jb * 256:DFF + (jb + 1) * 256], reads=[],
                  writes=[winBb[wb]])
            for jj in range(2):
                j = jb * 2 + jj
                for half in range(2):
                    pa, pab = C.ps[half], C.psb[half]
                    pb, pbb = C.ps[2 + half], C.psb[2 + half]
                    for dc in range(8):
                        P.op("pe", lambda e, pa=pa, wb=wb, dc=dc, jj=jj, half=half: e.matmul(
                            pa[:, :], winA[wb][:, dc, jj * 128:(jj + 1) * 128], hT[:, dc, half * 512:(half + 1) * 512],
                            start=(dc == 0), stop=(dc == 7)),
                            reads=[winAb[wb]] + hTb[half * 4:half * 4 + 4], writes=[pab])
                    for dc in range(8):
                        P.op("pe", lambda e, pb=pb, wb=wb, dc=dc, jj=jj, half=half: e.matmul(
                            pb[:, :], winB[wb][:, dc, jj * 128:(jj + 1) * 128], hT[:, dc, half * 512:(half + 1) * 512],
                            start=(dc == 0), stop=(dc == 7)),
                            reads=[winBb[wb]] + hTb[half * 4:half * 4 + 4], writes=[pbb])
                    P.op("act", lambda e, pa=pa, half=half: e.activation(out=sg[half][:, :], in_=pa[:, :], func=AF.Silu),
                         reads=[pab], writes=[sgb[half]])
                    P.op("dve", lambda e, pb=pb, half=half, j=j: e.tensor_tensor(
                        out=gT[:, j, half * 512:(half + 1) * 512], in0=sg[half][:, :], in1=pb[:, :], op=ALU.mult),
                        reads=[sgb[half], pbb], writes=[gTb[j][half]])
        for i in range(8):
            tt = tiles[i]
            for n in range(2):
                py, pyb = C.ps[4 + n], C.psb[4 + n]
                for fc in range(22):
                    P.op("pe", lambda e, py=py, fc=fc, i=i, n=n: e.matmul(
                        py[:, :], gT[:, fc, i * 128:(i + 1) * 128], wout[:, fc, n * 512:(n + 1) * 512],
                        start=(fc == 0), stop=(fc == 21)),
                        reads=[gTb[fc][i // 4], woutb[fc // 11]], writes=[pyb])
                P.op("dve", lambda e, py=py, i=i, n=n: e.scalar_tensor_tensor(
                    out=xt[:, i, n * 512:(n + 1) * 512], in0=py[:, :], scalar=0.5, in1=xt[:, i, n * 512:(n + 1) * 512],
                    op0=ALU.mult, op1=ALU.add),
                    reads=[pyb, xtb[i]], writes=[xtb[i]])
            if final_g is None:
                P.dma("sp", dst[tt * 128:(tt + 1) * 128, :], xt[:, i, :], reads=[xtb[i]], writes=[xbuf])
            else:
                final_norm_tile(P, C, xt, xtb, i, tt, final_g, out_dram)
    P.barrier()


def final_norm_tile(P, C, xt, xtb, i, tt, fg, out_dram):
    k = i % 2
    P.op("act", lambda e, i=i, k=k: e.activation(out=C.junk[k][:, :], in_=xt[:, i, :], func=AF.Square,
                                                 accum_out=C.ss[:, k:k + 1]),
         reads=[xtb[i]], writes=[C.junkb[k], C.ssb[k]])
    P.op("dve", lambda e, k=k: e.tensor_scalar(out=C.ss2[:, k:k + 1], in0=C.ss[:, k:k + 1], scalar1=1.0 / D,
                                               scalar2=EPS, op0=ALU.mult, op1=ALU.add),
         reads=[C.ssb[k]], writes=[C.ss2b[k]])
    P.op("act", lambda e, k=k: e.activation(out=C.ss3[:, k:k + 1], in_=C.ss2[:, k:k + 1], func=AF.Sqrt),
         reads=[C.ss2b[k]], writes=[C.ss3b[k]])
    P.op("dve", lambda e, k=k: e.reciprocal(out=C.rstd[:, k:k + 1], in_=C.ss3[:, k:k + 1]),
         reads=[C.ss3b[k]], writes=[C.rstdb[k]])
    P.op("dve", lambda e, i=i, k=k: e.scalar_tensor_tensor(out=xt[:, i, :], in0=xt[:, i, :],
                                                           scalar=C.rstd[:, k:k + 1], in1=C.fgB[:, :],
                                                           op0=ALU.mult, op1=ALU.mult),
         reads=[xtb[i], C.rstdb[k], C.fgBb], writes=[xtb[i]])
    P.dma("sp", out_dram[tt * 128:(tt + 1) * 128, :], xt[:, i, :], reads=[xtb[i]], writes=[C.outbuf])


CF_TRI, CF_REV, CF_MASK4, CF_ONES, CF_POW, CF_HM, CF_E, CF_N = 0, 128, 256, 768, 896, 928, 930, 994


def host_ohvec():
    oh = np.zeros((32, 512), np.float32)
    for idx in range(512):
        d = idx - 128
        if d < 0:
            continue
        if d < 16:
            b = d
        else:
            v = np.float32(np.log(np.float32(max(d, 1)) / np.float32(16.0))) / np.float32(np.log(128.0 / 16.0)) * np.float32(16.0)
            b = min(16 + int(v), 31)
        oh[b, idx] = 1.0
    return oh


def host_consts():
    import ml_dtypes
    j = np.arange(128)[:, None]
    i = np.arange(128)[None, :]
    cF = np.zeros((128, CF_N), np.float32)
    cF[:, CF_TRI:CF_TRI + 128] = (j <= i)
    cF[:, CF_REV:CF_REV + 128] = (j > i)
    cF[:, CF_MASK4:CF_MASK4 + 512] = np.tile((j <= i).astype(np.float32), (1, 4))
    cF[:, CF_ONES:CF_ONES + 128] = 1.0
    for i_ in range(NBIS):
        cF[:, CF_POW + i_] = 2.0 ** -(i_ + 1)
    cF[:, CF_POW + NBIS] = 2.0 ** -(NBIS + 1)
    cF[0:64, CF_HM] = 1.0
    cF[64:128, CF_HM + 1] = 1.0
    cF[64, CF_E:CF_E + 64] = 1.0
    negtri = np.where(j > i, -30000.0, 0.0).astype(np.float32).astype(ml_dtypes.bfloat16)
    return cF, negtri


def load_consts(P, C):
    C.cF = P.sb([128, CF_N], F32, "cF")
    C.cFb = P.buf("cF")
    P.dma("sp", C.cF[:, :], C.cF_dram[:, :], reads=[], writes=[C.cFb])
    C.negtri = P.sb([128, 128], BF16, "negtri")
    C.negtrib = P.buf("negtri")
    P.dma("sp", C.negtri[:, :], C.negtri_dram[:, :], reads=[], writes=[C.negtrib])


def build_hT_full(P, C, src, srcbuf, g_dram):
    hT = P.sb([128, 8, S], BF16, "hT")
    hTb = P.bufs_n(32, "hT")
    C.mark = P.sb_off
    gB = P.sb([128, D], F32, "gB")
    gBb = P.buf("gB")
    P.dma("sp", gB[:, :], g_dram.partition_broadcast(128), reads=[], writes=[gBb])
    xt = P.sb([128, 4, D], F32, "xt")
    xtb = P.bufs_n(4, "xt")
    for grp in range(8):
        load_norm_transpose(P, C, src, srcbuf, gB, gBb, xt, xtb, hT[:, :, grp * 512:(grp + 1) * 512],
                            hTb[grp * 4:grp * 4 + 4], list(range(grp * 4, grp * 4 + 4)))
    P.barrier(keep=True)
    P.sb_off = C.mark
    return hT, hTb


def attention_head(P, C, kA, kAb, qA, qAb, vfn, vb, M, ptb, pts, out_cb, negsel=None, nearb=None, fox=False,
                   qss=range(8)):
    for qs in qss:
        po, pob = C.ps[4 + C.roto % 2], C.psb[4 + C.roto % 2]
        C.roto += 1
        nk = 4 * qs + 4
        if negsel is not None:
            nsfn, nsb = negsel(qs)
        for kc in range(nk):
            t_lo = max(qs * 512, kc * 128)
            N = (qs + 1) * 512 - t_lo
            c0 = t_lo - qs * 512
            r = C.rot % 3
            C.rot += 1
            ps_, psb_ = C.ps[r], C.psb[r]
            extra = []
            if negsel is not None:
                extra.append((C.ident[:, :], nsfn(kc)[:, c0:c0 + N], 0, N, [C.identb, nsb]))
            diag_in = kc * 128 >= qs * 512
            if fox and diag_in:
                extra.append((C.ident[:, :], C.negtri[:, :], 0, 128, [C.identb, C.negtrib]))
            if nearb is not None:
                nb_ap, nb_b = nearb
                if diag_in and kc % 4 != 3:
                    extra.append((C.ident[:, :], nb_ap[:, 0:256], 0, 256, [C.identb, nb_b]))
                elif diag_in:
                    extra.append((C.ident[:, :], nb_ap[:, 0:128], 0, 128, [C.identb, nb_b]))
                elif kc == 4 * qs - 1:
                    extra.append((C.ident[:, :], nb_ap[:, 128:256], 0, 128, [C.identb, nb_b]))
            op_mm(P, ps_[:, 0:N], kA[:, kc * 128:(kc + 1) * 128], qA[:, t_lo:t_lo + N], True, len(extra) == 0,
                  [kAb, qAb], [psb_])
            for ei, (l_, r_, a, n_, rb) in enumerate(extra):
                op_mm(P, ps_[:, a:a + n_], l_, r_, False, ei == len(extra) - 1, rb, [psb_])
            pr = C.rotp % len(pts)
            C.rotp += 1
            op_act(P, pts[pr][:, 0:N], ps_[:, 0:N], AF.Exp, [psb_], [ptb[pr]])
            op_mm(P, po[0:M, c0:c0 + N], vfn(kc), pts[pr][:, 0:N], kc == 0, kc == nk - 1, [vb, ptb[pr]], [pob])
        out_cb(qs, po, pob)


def attn_finalize(P, C, po, pob, dst_ap, dstbuf, fin):
    k = C.rotf % 2
    C.rotf += 1
    num, numb, bcS, bcSb, oS, oSb = fin["num"][k], fin["numb"][k], fin["bcS"][k], fin["bcSb"][k], fin["oS"][k], fin["oSb"][k]
    op_copy(P, "act", num[0:65, :], po[0:65, :], [pob], [numb])
    pb, pbb = C.ps[3], C.psb[3]
    op_mm(P, pb[0:64, :], C.cF[0:65, CF_E:CF_E + 64], num[0:65, :], True, True, [C.cFb, numb], [pbb])
    P.op("dve", lambda e: e.reciprocal(out=bcS[0:64, :], in_=pb[0:64, :]), reads=[pbb], writes=[bcSb])
    op_tt(P, "dve", oS[0:64, :], num[0:64, :], bcS[0:64, :], ALU.mult, [numb, bcSb], [oSb])
    P.dma("sp", dst_ap, oS[0:64, :], reads=[oSb], writes=[dstbuf])


def alloc_fin(P):
    fin = {}
    fin["num"] = [P.sb([65, 512], F32, "num") for _ in range(2)]
    fin["numb"] = P.bufs_n(2, "num")
    fin["bcS"] = [P.sb([64, 512], F32, "bcS") for _ in range(2)]
    fin["bcSb"] = P.bufs_n(2, "bcS")
    fin["oS"] = [P.sb([64, 512], BF16, "oS") for _ in range(2)]
    fin["oSb"] = P.bufs_n(2, "oS")
    return fin


def even_phase(P, C, src, xs, xbuf, g_dram, w_in, w_gate, b_gate, gla_g, fox_b, w_out, omixT_dram, fsplit_dram):
    alloc_common(P, C)
    load_consts(P, C)
    C = copy.copy(C)
    omb = P.buf("omixT_dram")
    fsb = P.buf("fsplit_dram")
    hT, hTb = build_hT_full(P, C, src, xbuf, g_dram)
    mark = C.mark
    w_in_v = w_in.rearrange("(dc p) n -> p dc n", p=128)
    omix_v = omixT_dram.rearrange("(c p) t -> p c t", p=128)

    wg = P.sb([128, 8, 1552], BF16, "wg")
    wgb = P.buf("wg")
    P.dma("pool", wg[:, :, :], w_in_v[:, :, 0:1552], reads=[], writes=[wgb])
    wgate = P.sb([16, 256], F32, "wgate")
    bgate = P.sb([1, 256], F32, "bgate")
    gwb = P.buf("gw")
    P.dma("sp", wgate[:, :], w_gate, reads=[], writes=[gwb])
    P.dma("sp", bgate[:, :], b_gate.rearrange("(o n) -> o n", o=1), reads=[], writes=[gwb])
    gnB = P.sb([128, 128], F32, "gnB")
    P.dma("sp", gnB[:, :], gla_g.partition_broadcast(128), reads=[], writes=[gwb])
    qTs = P.sb([128, 2, 512], F32, "qTs")
    kTs = P.sb([128, 2, 512], F32, "kTs")
    qTsb, kTsb = P.buf("qTs"), P.buf("kTs")
    gkl = P.sb([16, 512], F32, "gkl")
    gklb = P.buf("gkl")
    vtok = P.sb([128, 512], BF16, "vtok")
    vtokb = P.buf("vtok")
    ktok = P.sb([128, 256], F32, "ktok")
    ktokb = P.buf("ktok")
    sgo = P.sb([128, 512], F32, "sgo")
    sgob = P.buf("sgo")
    ez = P.sb([128, 256], F32, "ez")
    ezb = P.buf("ez")
    Lt = P.sb([128, 256], F32, "Lt")
    Ltb = P.buf("Lt")
    e1 = P.sb([128, 256], F32, "e1")
    e2 = P.sb([128, 256], F32, "e2")
    e3 = P.sb([128, 256], F32, "e3")
    e1b, e2b, e3b = P.buf("e1"), P.buf("e2"), P.buf("e3")
    qdT = P.sb([128, 2, 2, 128], BF16, "qdT")
    kdT = P.sb([128, 2, 2, 128], BF16, "kdT")
    qdTb, kdTb = P.buf("qdT"), P.buf("kdT")
    kte = P.sb([128, 256], BF16, "kte")
    kteb = P.buf("kte")
    AT = P.sb([128, 512], BF16, "AT")
    ATb = P.buf("AT")
    St = P.sb([128, 2, 128], F32, "St")
    Sbf = P.sb([128, 2, 128], BF16, "Sbf")
    Stb = P.bufs_n(2, "St")
    Sbfb = P.bufs_n(2, "Sbf")
    dec = P.sb([128, 2], F32, "dec")
    decb = P.buf("dec")
    osq = P.sb([128, 512], F32, "osq")
    osqb = P.buf("osq")
    sm = P.sb([128, 16], F32, "sm")
    smb = P.bufs_n(4, "sm")
    t1 = P.sb([128, 512], F32, "t1")
    t1b = P.buf("t1")
    og = P.sb([128, 512], BF16, "og")
    ogb = P.buf("og")
    ogT = [P.sb([128, 4, 128], BF16, "ogT") for _ in range(2)]
    ogTb = P.bufs_n(2, "ogT")
    P.op("dve", lambda e: e.memset(St[:, :, :], 0.0), reads=[], writes=Stb)
    ps = C.ps
    psb = C.psb
    for sb_ in (range(8) if "g" in PARTS else []):
        tsl = slice(sb_ * 512, (sb_ + 1) * 512)
        hb = hTb[sb_ * 4:sb_ * 4 + 4]
        for c in range(2):
            for dc in range(8):
                op_mm(P, ps[0][:, :], wg[:, dc, c * 128:(c + 1) * 128], hT[:, dc, tsl], dc == 0, dc == 7, [wgb] + hb, [psb[0]])
            op_act(P, qTs[:, c, :], ps[0][:, :], AF.Copy, [psb[0]], [qTsb], scale=0.125)
            for dc in range(8):
                op_mm(P, ps[1][:, :], wg[:, dc, 256 + c * 128:256 + (c + 1) * 128], hT[:, dc, tsl], dc == 0, dc == 7, [wgb] + hb, [psb[1]])
            op_copy(P, "dve", kTs[:, c, :], ps[1][:, :], [psb[1]], [kTsb])
        for dc in range(8):
            op_mm(P, ps[2][0:16, :], wg[:, dc, 1536:1552], hT[:, dc, tsl], dc == 0, dc == 7, [wgb] + hb, [psb[2]])
        op_copy(P, "act", gkl[:, :], ps[2][0:16, :], [psb[2]], [gklb])
        for ch in range(4):
            tt = sb_ * 4 + ch
            ttl = slice(tt * 128, (tt + 1) * 128)
            csl = slice(ch * 128, (ch + 1) * 128)
            if GLA_STOP < 4:
                continue
            for dc in range(8):
                op_mm(P, ps[0][:, 0:256], hT[:, dc, ttl], wg[:, dc, 256:512], dc == 0, dc == 7, [wgb, hTb[tt]], [psb[0]])
            for dc in range(8):
                op_mm(P, ps[1][:, :], hT[:, dc, ttl], wg[:, dc, 512:1024], dc == 0, dc == 7, [wgb, hTb[tt]], [psb[1]])
            op_copy(P, "act", vtok[:, :], ps[1][:, :], [psb[1]], [vtokb])
            op_copy(P, "act", ktok[:, :], ps[0][:, 0:256], [psb[0]], [ktokb])
            for dc in range(8):
                op_mm(P, ps[2][:, :], hT[:, dc, ttl], wg[:, dc, 1024:1536], dc == 0, dc == 7, [wgb, hTb[tt]], [psb[2]])
            op_act(P, sgo[:, :], ps[2][:, :], AF.Silu, [psb[2]], [sgob])
            if GLA_STOP < 5:
                continue
            op_mm(P, ps[3][:, 0:256], gkl[0:16, csl], wgate[0:16, :], True, False, [gklb, gwb], [psb[3]])
            op_mm(P, ps[3][:, 0:256], C.cF[0:1, CF_ONES:CF_ONES + 128], bgate[0:1, :], False, True, [C.cFb, gwb], [psb[3]])
            op_act(P, ez[:, :], ps[3][:, 0:256], AF.Exp, [psb[3]], [ezb], scale=-1.0)
            op_act(P, Lt[:, :], ez[:, :], AF.Ln, [ezb], [Ltb], bias=1.0)
            if GLA_STOP < 6:
                continue
            for c in range(2):
                op_mm(P, ps[3][:, 256 + c * 128:256 + (c + 1) * 128], Lt[:, c * 128:(c + 1) * 128],
                      C.cF[:, CF_TRI:CF_TRI + 128], True, True, [Ltb, C.cFb], [psb[3]])
            if GLA_STOP < 6.2:
                continue
            op_act(P, e1[:, :], ps[3][:, 256:512], AF.Exp, [psb[3]], [e1b], scale=-1.0 / 16)
            op_act(P, e2[:, :], ps[3][:, 256:512], AF.Exp, [psb[3]], [e2b], scale=1.0 / 16)
            if GLA_STOP < 6.3:
                continue
            for hh in range(2):
                op_stt(P, qdT[:, hh, :, :], qTs[:, :, csl], C.cF[:, CF_HM + hh:CF_HM + hh + 1],
                       e1[:, :].rearrange("p (c t) -> p c t", c=2), ALU.mult, ALU.mult, [qTsb, e1b, C.cFb], [qdTb])
                op_stt(P, kdT[:, hh, :, :], kTs[:, :, csl], C.cF[:, CF_HM + hh:CF_HM + hh + 1],
                       e2[:, :].rearrange("p (c t) -> p c t", c=2), ALU.mult, ALU.mult, [kTsb, e2b, C.cFb], [kdTb])
            if GLA_STOP < 6.4:
                continue
            op_copy(P, "dve", dec[:, :], e1[:, :].rearrange("p (c t) -> p c t", c=2)[:, :, 127], [e1b], [decb])
            if GLA_STOP < 7:
                continue
            for c in range(2):
                op_mm(P, ps[4][:, c * 128:(c + 1) * 128], C.cF[:, CF_REV:CF_REV + 128], Lt[:, c * 128:(c + 1) * 128], True, True,
                      [Ltb, C.cFb], [psb[4]])
            if GLA_STOP < 7.1:
                continue
            op_act(P, e3[:, :], ps[4][:, 0:256], AF.Exp, [psb[4]], [e3b], scale=-1.0 / 16)
            if GLA_STOP < 7.2:
                continue
            op_tt(P, "pool", kte[:, :], ktok[:, :], e3[:, :], ALU.mult, [ktokb, e3b], [kteb])
            if GLA_STOP < 8:
                continue
            for h in range(4):
                op_mm(P, ps[5][:, h * 128:(h + 1) * 128], kdT[:, h % 2, h // 2, :], qdT[:, h % 2, h // 2, :],
                      True, True, [kdTb, qdTb], [psb[5]])
            op_tt(P, "dve", AT[:, :], ps[5][:, :], C.cF[:, CF_MASK4:CF_MASK4 + 512], ALU.mult, [psb[5], C.cFb], [ATb])
            if GLA_STOP < 9:
                continue
            for h in range(4):
                po_ = (h % 2) * 64
                first = (tt == 0)
                op_mm(P, ps[1][:, h * 128:(h + 1) * 128], AT[:, h * 128:(h + 1) * 128], vtok[:, h * 128:(h + 1) * 128],
                      True, first, [ATb, vtokb], [psb[1]])
                if not first:
                    op_mm(P, ps[1][:, h * 128:(h + 1) * 128], qdT[:, h % 2, h // 2, :], Sbf[:, h // 2, :],
                          False, True, [qdTb, Sbfb[h // 2]], [psb[1]])
            for c in range(2):
                op_mm(P, ps[4][:, 256:512], kte[:, c * 128:(c + 1) * 128], vtok[:, c * 256:(c + 1) * 256], True, True,
                      [kteb, vtokb], [psb[4]])
                for hh in range(2):
                    pp = slice(hh * 64, hh * 64 + 64)
                    op_stt(P, St[pp, c, :], St[pp, c, :], dec[pp, c:c + 1], ps[4][pp, 256 + hh * 128:256 + (hh + 1) * 128],
                           ALU.mult, ALU.add, [Stb[c], decb, psb[4]], [Stb[c]])
                op_copy(P, "act", Sbf[:, c, :], St[:, c, :], [Stb[c]], [Sbfb[c]])
            if GLA_STOP < 10:
                continue
            op_act(P, osq[:, :], ps[1][:, :], AF.Square, [psb[1]], [osqb])
            P.op("dve", lambda e: e.tensor_reduce(out=sm[:, 0:4], in_=osq[:, :].rearrange("p (h v) -> p h v", h=4),
                                                  axis=AX.X, op=ALU.add), reads=[osqb], writes=[smb[0]])
            op_ts(P, "dve", sm[:, 4:8], sm[:, 0:4], 1.0 / 128, EPS, ALU.mult, ALU.add, [smb[0]], [smb[1]])
            op_act(P, sm[:, 8:12], sm[:, 4:8], AF.Sqrt, [smb[1]], [smb[2]])
            P.op("dve", lambda e: e.reciprocal(out=sm[:, 12:16], in_=sm[:, 8:12]), reads=[smb[2]], writes=[smb[3]])
            for h in range(4):
                op_stt(P, t1[:, h * 128:(h + 1) * 128], ps[1][:, h * 128:(h + 1) * 128], sm[:, 12 + h:13 + h], gnB[:, :],
                       ALU.mult, ALU.mult, [psb[1], smb[3], gwb], [t1b])
            op_tt(P, "dve", og[:, :], t1[:, :], sgo[:, :], ALU.mult, [t1b, sgob], [ogb])
            k = tt % 2
            for h in range(4):
                P.op("pe", lambda e, h=h, k=k: e.transpose(out=C.psT[k][:, h * 128:(h + 1) * 128],
                                                           in_=og[:, h * 128:(h + 1) * 128], identity=C.ident[:, :]),
                     reads=[ogb, C.identb], writes=[C.psTb[k]])
            op_copy(P, "act", ogT[k][:, :, :], C.psT[k][:, 0:512].rearrange("p (c t) -> p c t", c=4), [C.psTb[k]], [ogTb[k]])
            P.dma("sp", omix_v[:, 0:4, ttl], ogT[k][:, :, :], reads=[ogTb[k]], writes=[omb])

    P.barrier(keep=True)
    P.sb_off = mark
    wf = P.sb([128, 8, 1544], BF16, "wf")
    wfb = P.buf("wf")
    P.dma("pool", wf[:, :, :], w_in_v[:, :, 1552:3096], reads=[], writes=[wfb])
    vaug = P.sb([128, 32, 8, 65], BF16, "vaug")
    vaugb = P.buf("vaug")
    P.op("pool", lambda e: e.memset(vaug[:, :, :, 64:65], 1.0), reads=[], writes=[vaugb])
    for tt in (range(32) if "v" in PARTS else []):
        r = tt % 3
        for dc in range(8):
            op_mm(P, ps[r][:, :], hT[:, dc, tt * 128:(tt + 1) * 128], wf[:, dc, 1024:1536], dc == 0, dc == 7,
                  [wfb, hTb[tt]], [psb[r]])
        op_copy(P, "act" if tt % 2 else "dve", vaug[:, tt, :, 0:64], ps[r][:, :].rearrange("p (h d) -> p h d", h=8),
                [psb[r]], [vaugb])
    mark2 = P.sb_off
    fb = P.sb([8, 2], F32, "fb")
    fbb = P.buf("fb")
    P.dma("sp", fb[:, 0:1], fox_b.rearrange("(h o) -> h o", o=1), reads=[], writes=[fbb])
    op_ts(P, "dve", fb[:, 1:2], fb[:, 0:1], -1.0, None, ALU.mult, None, [fbb], [fbb])
    zer = P.sb([8, 1024], F32, "zer")
    zerb = P.buf("zer")
    P.op("pool", lambda e: e.memset(zer[:, :], 0.0), reads=[], writes=[zerb])
    lT = P.sb([8, 1024], F32, "lT")
    lTb = P.buf("lT")
    fe = P.sb([8, 512], F32, "fe")
    feb = P.buf("fe")
    Fp = P.sb([8, 1024], F32, "Fp")
    Fpb = P.buf("Fp")
    r1 = P.sb([8, 1024], F32, "r1")
    r2 = P.sb([8, 1024], F32, "r2")
    r1b, r2b = P.buf("r1"), P.buf("r2")
    spl = P.sb([8, 6, 1024], BF16, "spl")
    splb = P.buf("spl")
    carry = P.sb([8, 1], F32, "carry")
    carryb = P.buf("carry")
    P.op("dve", lambda e: e.memset(carry[:, :], 0.0), reads=[], writes=[carryb])
    for pc in (range(4) if "s" in PARTS else []):
        for s2 in range(2):
            sb_ = pc * 2 + s2
            for dc in range(8):
                op_mm(P, ps[3][0:8, :], wf[:, dc, 1536:1544], hT[:, dc, sb_ * 512:(sb_ + 1) * 512], dc == 0, dc == 7,
                      [wfb] + hTb[sb_ * 4:sb_ * 4 + 4], [psb[3]])
            op_act(P, fe[:, :], ps[3][0:8, :], AF.Exp, [psb[3], fbb], [feb], scale=-1.0, bias=fb[:, 1:2])
            op_act(P, lT[:, s2 * 512:(s2 + 1) * 512], fe[:, :], AF.Ln, [feb], [lTb], bias=1.0)
        P.op("dve", lambda e: e.tensor_tensor_scan(out=Fp[:, :], data0=lT[:, :], data1=zer[:, :], initial=carry[:, 0:1],
                                                   op0=ALU.add, op1=ALU.add), reads=[lTb, zerb, carryb], writes=[Fpb])
        op_copy(P, "dve", carry[:, :], Fp[:, 1023:1024], [Fpb], [carryb])
        op_copy(P, "dve", spl[:, 3, :], Fp[:, :], [Fpb], [splb])
        op_tt(P, "dve", r1[:, :], Fp[:, :], spl[:, 3, :], ALU.subtract, [Fpb, splb], [r1b])
        op_copy(P, "dve", spl[:, 4, :], r1[:, :], [r1b], [splb])
        op_tt(P, "dve", r2[:, :], r1[:, :], spl[:, 4, :], ALU.subtract, [r1b, splb], [r2b])
        op_copy(P, "dve", spl[:, 5, :], r2[:, :], [r2b], [splb])
        op_ts(P, "dve", spl[:, 0:3, :], spl[:, 3:6, :], -1.0, None, ALU.mult, None, [splb], [splb])
        for side in range(2):
            P.dma("sp", fsplit_dram[side, :, :, pc * 1024:(pc + 1) * 1024], spl[:, side * 3:side * 3 + 3, :],
                  reads=[splb], writes=[fsb])
    P.barrier(keep=True)
    P.sb_off = mark2
    qa = [P.sb([70, S], BF16, "qa") for _ in range(2)]
    ka = [P.sb([70, S], BF16, "ka") for _ in range(2)]
    qab, kab = P.bufs_n(2, "qa"), P.bufs_n(2, "ka")
    pts = [P.sb([128, 512], BF16, "pt") for _ in range(3)]
    ptb = P.bufs_n(3, "pt")
    fin = alloc_fin(P)
    C.rot = C.rotp = C.rotf = 0
    for h in (range(8) if "h" in PARTS else []):
        k = h % 2
        P.op("pool", lambda e, k=k: e.memset(qa[k][64:70, :], 1.0), reads=[], writes=[qab[k]])
        P.op("pool", lambda e, k=k: e.memset(ka[k][64:70, :], 1.0), reads=[], writes=[kab[k]])
        P.dma("sp", qa[k][64:67, :], fsplit_dram[0, h, :, :], reads=[fsb], writes=[qab[k]])
        P.dma("sp", ka[k][67:70, :], fsplit_dram[1, h, :, :], reads=[fsb], writes=[kab[k]])
        for sb_ in range(8):
            tsl = slice(sb_ * 512, (sb_ + 1) * 512)
            hb = hTb[sb_ * 4:sb_ * 4 + 4]
            r = C.rot % 3
            C.rot += 1
            for dc in range(8):
                op_mm(P, ps[r][0:64, :], wf[:, dc, h * 64:(h + 1) * 64], hT[:, dc, tsl], dc == 0, dc == 7, [wfb] + hb, [psb[r]])
            op_act(P, qa[k][0:64, tsl], ps[r][0:64, :], AF.Copy, [psb[r]], [qab[k]], scale=0.125)
            r = C.rot % 3
            C.rot += 1
            for dc in range(8):
                op_mm(P, ps[r][0:64, :], wf[:, dc, 512 + h * 64:512 + (h + 1) * 64], hT[:, dc, tsl], dc == 0, dc == 7,
                      [wfb] + hb, [psb[r]])
            op_copy(P, "dve", ka[k][0:64, tsl], ps[r][0:64, :], [psb[r]], [kab[k]])

        def out_cb(qs, po, pob, h=h):
            attn_finalize(P, C, po, pob, omixT_dram[512 + h * 64:512 + (h + 1) * 64, qs * 512:(qs + 1) * 512], omb, fin)
        attention_head(P, C, ka[k][0:70, :], kab[k], qa[k][0:70, :], qab[k], lambda kc, h=h: vaug[:, kc, h, :], vaugb,
                       65, ptb, pts, out_cb, fox=True)

    P.barrier(keep=True)
    P.sb_off = mark
    if "p" in PARTS:
        mixer_out_proj(P, C, src, xs, xbuf, omixT_dram, omb, w_out)
    P.barrier()


def mixer_out_proj(P, C, src, xs, xbuf, omixT_dram, omb, w_out):
    ps, psb = C.ps, C.psb
    om = P.sb([128, 8, S], BF16, "om")
    omsb = P.bufs_n(8, "om")
    omix_v = omixT_dram.rearrange("(c p) t -> p c t", p=128)
    for c in range(8):
        P.dma("sp", om[:, c, :], omix_v[:, c, :], reads=[omb], writes=[omsb[c]])
    wo = P.sb([128, 8, D], BF16, "wo")
    wob = P.buf("wo")
    P.dma("pool", wo[:, :, :], w_out.rearrange("(c p) n -> p c n", p=128), reads=[], writes=[wob])
    xt = P.sb([128, 4, D], F32, "xt2")
    xtb = P.bufs_n(4, "xt2")
    for tt in range(32):
        i = tt % 4
        P.dma("sp", xt[:, i, :], src[tt * 128:(tt + 1) * 128, :], reads=[xbuf], writes=[xtb[i]])
        for n in range(2):
            r = (tt * 2 + n) % 4
            for c in range(8):
                op_mm(P, ps[r][:, :], om[:, c, tt * 128:(tt + 1) * 128], wo[:, c, n * 512:(n + 1) * 512], c == 0, c == 7,
                      [omsb[c], wob], [psb[r]])
            op_tt(P, "dve", xt[:, i, n * 512:(n + 1) * 512], ps[r][:, :], xt[:, i, n * 512:(n + 1) * 512], ALU.add,
                  [psb[r], xtb[i]], [xtb[i]])
        P.dma("sp", xs[tt * 128:(tt + 1) * 128, :], xt[:, i, :], reads=[xtb[i]], writes=[xbuf])


NBIS = 16


def odd_phase(P, C, src, xs, xbuf, g_dram, w_in, kv_g, w_uk, w_uv, w_out, t5, omixT_dram, nsT_dram, qT_dram):
    alloc_common(P, C)
    load_consts(P, C)
    C = copy.copy(C)
    ps, psb = C.ps, C.psb
    omb = P.buf("omixT_dram")
    nsb_d = P.buf("nsT_dram")
    qTb_d = P.buf("qT_dram")
    w_in_v = w_in.rearrange("(dc p) n -> p dc n", p=128)
    cT = P.sb([128, 2, S], BF16, "cT")
    cTb = P.bufs_n(32, "cT")
    mark0 = P.sb_off
    kidx2 = P.sb([128, S], BF16, "kidx2")
    kidx2b = P.buf("kidx2")
    qidx = P.sb([128, 4, S], BF16, "qidx")
    qidxb = P.bufs_n(8, "qidx")
    wq = P.sb([128, 32, 8], F32, "wq")
    wqb = P.bufs_n(32, "wq")
    hT, hTb = build_hT_full(P, C, src, xbuf, g_dram)
    mark1 = C.mark
    if ODD_STOP < 1:
        P.barrier()
        return
    wA = P.sb([128, 8, 840], BF16, "wA")
    wAb = P.buf("wA")
    P.dma("pool", wA[:, :, :], w_in_v[:, :, 1024:1864], reads=[], writes=[wAb])
    wQ = P.sb([128, 8, 1024], BF16, "wQ")
    wQb = P.buf("wQ")
    P.dma("pool", wQ[:, :, :], w_in_v[:, :, 0:1024], reads=[], writes=[wQb])
    wk2 = P.sb([128, 8, 128], BF16, "wk2")
    wk2b = P.buf("wk2")
    P.dma("pool", wk2[:, :, 0:64], w_in_v[:, :, 1792:1856], reads=[], writes=[wk2b])
    P.dma("pool", wk2[:, :, 64:128], w_in_v[:, :, 1792:1856], reads=[], writes=[wk2b])
    gkv = P.sb([128, 256], F32, "gkv")
    gkvb = P.buf("gkv")
    P.dma("sp", gkv[:, :], kv_g.partition_broadcast(128), reads=[], writes=[gkvb])
    cjunk = P.sb([128, 256], BF16, "cjunk")
    cjunkb = P.buf("cjunk")
    cs = P.sb([128, 8], F32, "cs")
    csb = P.bufs_n(4, "cs")
    ctok = [P.sb([128, 256], BF16, "ctok") for _ in range(2)]
    ctokb = P.bufs_n(2, "ctok")
    qst = [P.sb([128, 512], BF16, "qst") for _ in range(2)]
    qstb = P.bufs_n(2, "qst")
    rr = 0
    for sb_ in range(8):
        tsl = slice(sb_ * 512, (sb_ + 1) * 512)
        hb = hTb[sb_ * 4:sb_ * 4 + 4]
        r = rr % 4
        rr += 1
        for dc in range(8):
            op_mm(P, ps[r][:, :], wk2[:, dc, :], hT[:, dc, tsl], dc == 0, dc == 7, [wk2b] + hb, [psb[r]])
        op_copy(P, "act", kidx2[:, tsl], ps[r][:, :], [psb[r]], [kidx2b])
        for p4 in range(4):
            r = rr % 4
            rr += 1
            for dc in range(8):
                op_mm(P, ps[r][:, :], wA[:, dc, 256 + p4 * 128:256 + (p4 + 1) * 128], hT[:, dc, tsl], dc == 0, dc == 7,
                      [wAb] + hb, [psb[r]])
            op_copy(P, "dve" if p4 % 2 else "act", qidx[:, p4, tsl], ps[r][:, :], [psb[r]], [qidxb[sb_]])
        for pr in range(8):
            r = rr % 4
            rr += 1
            for dc in range(8):
                op_mm(P, ps[r][:, :], wQ[:, dc, pr * 128:(pr + 1) * 128], hT[:, dc, tsl], dc == 0, dc == 7, [wQb] + hb, [psb[r]])
            k = pr % 2
            op_act(P, qst[k][:, :], ps[r][:, :], AF.Copy, [psb[r]], [qstb[k]], scale=0.125)
            P.dma("sp", qT_dram[pr * 128:(pr + 1) * 128, tsl], qst[k][:, :], reads=[qstb[k]], writes=[qTb_d])
        for ch in range(4):
            tt = sb_ * 4 + ch
            ttl = slice(tt * 128, (tt + 1) * 128)
            k = tt % 2
            for dc in range(8):
                op_mm(P, ps[4][:, 0:256], hT[:, dc, ttl], wA[:, dc, 0:256], dc == 0, dc == 7, [wAb, hTb[tt]], [psb[4]])
            for dc in range(8):
                op_mm(P, ps[5][:, 0:128], hT[:, dc, ttl], wA[:, dc, 712:840], dc == 0, dc == 7, [wAb, hTb[tt]], [psb[5]])
            op_copy(P, "dve", wq[:, tt, :], ps[5][:, 120:128], [psb[5]], [wqb[tt]])
            op_act(P, cjunk[:, :], ps[4][:, 0:256], AF.Square, [psb[4]], [cjunkb, csb[0]], accum_out=cs[:, 0:1])
            op_ts(P, "dve", cs[:, 1:2], cs[:, 0:1], 1.0 / 256, EPS, ALU.mult, ALU.add, [csb[0]], [csb[1]])
            op_act(P, cs[:, 2:3], cs[:, 1:2], AF.Sqrt, [csb[1]], [csb[2]])
            P.op("dve", lambda e: e.reciprocal(out=cs[:, 3:4], in_=cs[:, 2:3]), reads=[csb[2]], writes=[csb[3]])
            op_stt(P, ctok[k][:, :], ps[4][:, 0:256], cs[:, 3:4], gkv[:, :], ALU.mult, ALU.mult, [psb[4], csb[3], gkvb], [ctokb[k]])
            for lc in range(2):
                P.op("pe", lambda e, lc=lc, k=k: e.transpose(out=C.psT[k][:, lc * 128:(lc + 1) * 128],
                                                             in_=ctok[k][:, lc * 128:(lc + 1) * 128], identity=C.ident[:, :]),
                     reads=[ctokb[k], C.identb], writes=[C.psTb[k]])
            op_copy(P, "act", cT[:, :, ttl], C.psT[k][:, 0:256].rearrange("p (c t) -> p c t", c=2), [C.psTb[k]], [cTb[tt]])
    if ODD_STOP < 2:
        P.barrier()
        return
    P.barrier(keep=True)
    P.sb_off = mark1
    sc = [P.sb([128, S], F32, "sc") for _ in range(2)]
    scb = P.bufs_n(2, "sc")
    cjk = P.sb([128, S], BF16, "cjk")
    cjkb = P.buf("cjk")
    nsel = P.sb([128, S], BF16, "nsel")
    nselb = P.buf("nsel")
    nst = P.sb([128, 32, 128], BF16, "nst")
    nstb = P.buf("nst")
    rl = [P.sb([128, 512], F32, "rl") for _ in range(3)]
    rlb = P.bufs_n(3, "rl")
    bs = P.sb([128, 8 + NBIS + 1], F32, "bs")
    bsb = P.bufs_n(8, "bs")
    stepb = P.buf("step")
    qm = P.sb([128, 8, 128], BF16, "qm")
    qmb = P.buf("qm")
    zt = P.sb([128, 128], BF16, "zt")
    ztb = P.buf("zt")
    P.op("pool", lambda e: e.memset(zt[:, :], 0.0), reads=[], writes=[ztb])
    P.dma("sp", nsT_dram[0, :, 0:128], C.negtri[:, :], reads=[C.negtrib], writes=[nsb_d])
    P.dma("sp", nsT_dram[0, :, 128:256], zt[:, :], reads=[ztb], writes=[nsb_d])
    P.dma("sp", nsT_dram[1, :, 128:256], C.negtri[:, :], reads=[C.negtrib], writes=[nsb_d])
    rr = 0
    for qc in (range(2, 32) if "2" in OPARTS else []):
        L = (qc + 1) * 128
        qsl = slice(qc * 128, (qc + 1) * 128)
        k = qc % 2
        s_, s_b = sc[k], scb[k]
        nck = (L + 511) // 512
        for h in range(8):
            op_ts(P, "pool", qm[:, h, :], qidx[:, h // 2, qsl], C.cF[:, CF_HM + h % 2:CF_HM + h % 2 + 1], None, ALU.mult, None,
                  [qidxb[qc // 4], C.cFb], [qmb])
        for ck in range(nck):
            n = min(512, L - ck * 512)
            for h in range(8):
                po_ = (h % 2) * 64
                r = rr % 4
                rr += 1
                op_mm(P, ps[r][:, 0:n], qm[:, h, :], kidx2[:, ck * 512:ck * 512 + n], True, True,
                      [qmb, kidx2b], [psb[r]])
                r3 = rr % 3
                op_act(P, rl[r3][:, 0:n], ps[r][:, 0:n], AF.Relu, [psb[r]], [rlb[r3]])
                if h == 0:
                    op_ts(P, "dve", s_[:, ck * 512:ck * 512 + n], rl[r3][:, 0:n], wq[:, qc, 0:1], None, ALU.mult, None,
                          [rlb[r3], wqb[qc]], [s_b])
                else:
                    op_stt(P, s_[:, ck * 512:ck * 512 + n], rl[r3][:, 0:n], wq[:, qc, h:h + 1], s_[:, ck * 512:ck * 512 + n],
                           ALU.mult, ALU.add, [rlb[r3], wqb[qc], s_b], [s_b])
        P.op("pool", lambda e, s_=s_, L=L: e.affine_select(out=s_[:, L - 128:L], in_=s_[:, L - 128:L], pattern=[[-1, 128]],
                                                            compare_op=ALU.is_ge, fill=-1.0e30, base=0, channel_multiplier=1),
             reads=[s_b], writes=[s_b])
        P.op("dve", lambda e, s_=s_, L=L: e.tensor_reduce(out=bs[:, 0:1], in_=s_[:, 0:L], axis=AX.X, op=ALU.max),
             reads=[s_b], writes=[bsb[0]])
        P.op("dve", lambda e, s_=s_, L=L: e.tensor_reduce(out=bs[:, 1:2], in_=s_[:, 0:L - 128], axis=AX.X, op=ALU.min),
             reads=[s_b], writes=[bsb[1]])
        op_tt(P, "dve", bs[:, 2:3], bs[:, 0:1], bs[:, 1:2], ALU.subtract, [bsb[0], bsb[1]], [bsb[2]])
        op_ts(P, "dve", bs[:, 8:8 + NBIS + 1], C.cF[:, CF_POW:CF_POW + NBIS + 1], bs[:, 2:3], None, ALU.mult, None,
              [bsb[2], C.cFb], [stepb])
        op_stt(P, bs[:, 3:4], bs[:, 2:3], 0.5, bs[:, 1:2], ALU.mult, ALU.add, [bsb[2], bsb[1]], [bsb[3]])
        for it in range(NBIS):
            P.op("dve", lambda e, s_=s_, L=L: e.tensor_scalar(out=cjk[:, 0:L], in0=s_[:, 0:L], scalar1=bs[:, 3:4], scalar2=None,
                                                               op0=ALU.is_ge, op1=ALU.add, accum_out=bs[:, 4:5]),
                 reads=[s_b, bsb[3]], writes=[cjkb, bsb[4]])
            op_ts(P, "dve", bs[:, 5:6], bs[:, 4:5], 255.5, 0.5, ALU.is_ge, ALU.subtract, [bsb[4]], [bsb[5]])
            op_stt(P, bs[:, 3:4], bs[:, 8 + it:9 + it], bs[:, 5:6], bs[:, 3:4], ALU.mult, ALU.add, [stepb, bsb[5], bsb[3]], [bsb[3]])
        op_tt(P, "dve", bs[:, 6:7], bs[:, 3:4], bs[:, 8 + NBIS:9 + NBIS], ALU.subtract, [bsb[3], stepb], [bsb[6]])
        P.op("pool", lambda e, s_=s_, L=L: e.tensor_scalar(out=nsel[:, 0:L], in0=s_[:, 0:L], scalar1=bs[:, 6:7], scalar2=-30000.0,
                                                            op0=ALU.is_lt, op1=ALU.mult),
             reads=[s_b, bsb[6]], writes=[nselb])
        for g in range((qc + 8) // 8):
            kk = g % 2
            nb_ = min(8, qc + 1 - g * 8)
            for j in range(nb_):
                kc = g * 8 + j
                P.op("pe", lambda e, kc=kc, j=j, kk=kk: e.transpose(out=C.psT[kk][:, j * 128:(j + 1) * 128],
                                                                    in_=nsel[:, kc * 128:(kc + 1) * 128], identity=C.ident[:, :]),
                     reads=[nselb, C.identb], writes=[C.psTb[kk]])
            op_copy(P, "act", nst[:, g * 8:g * 8 + nb_, :], C.psT[kk][:, 0:nb_ * 128].rearrange("p (c t) -> p c t", c=nb_),
                    [C.psTb[kk]], [nstb])
        P.dma("sp", nsT_dram[0:qc + 1, :, qsl].rearrange("k s t -> s k t"), nst[:, 0:qc + 1, :], reads=[nstb], writes=[nsb_d])
    if ODD_STOP < 3:
        P.barrier()
        return
    P.barrier(keep=True)
    P.sb_off = mark0
    wukf = P.sb([128, 8, 256], F32, "wukf")
    wukfb = P.buf("wukf")
    P.dma("sp", wukf[:, :, :], w_uk.rearrange("(pr hh) d l -> (hh d) pr l", hh=2), reads=[], writes=[wukfb])
    wukT = P.sb([128, 2, 8, 128], BF16, "wukT")
    wukTb = P.buf("wukT")
    for pr in (range(8) if "a" in OPARTS else []):
        for lc in range(2):
            r = (pr * 2 + lc) % 4
            P.op("pe", lambda e, pr=pr, lc=lc, r=r: e.transpose(out=ps[r][:, 0:128], in_=wukf[:, pr, lc * 128:(lc + 1) * 128],
                                                                identity=C.identf[:, :]),
                 reads=[wukfb, C.identb], writes=[psb[r]])
            op_copy(P, "act" if lc else "dve", wukT[:, lc, pr, :], ps[r][:, 0:128], [psb[r]], [wukTb])
    wuv = P.sb([128, 2, 16, 64], BF16, "wuv")
    wuvb = P.buf("wuv")
    for lc in range(2):
        P.dma("pool", wuv[:, lc, :, :], w_uv[:, lc * 128:(lc + 1) * 128, :].rearrange("h p d -> p h d"), reads=[], writes=[wuvb])
    if ODD_STOP < 4:
        P.barrier()
        return
    tab = P.sb([32, 16], F32, "tab")
    tab31 = P.sb([32, 16], F32, "tab31")
    tabb = P.buf("tab")
    P.dma("sp", tab[:, :], t5, reads=[], writes=[tabb])
    P.dma("sp", tab31[:, :], t5[31, :].partition_broadcast(32), reads=[], writes=[tabb])
    tab2 = P.sb([32, 16], F32, "tab2")
    tab2b = P.buf("tab2")
    op_tt(P, "dve", tab2[:, :], tab[:, :], tab31[:, :], ALU.subtract, [tabb], [tab2b])
    ohr = P.sb([32, 512], F32, "ohr")
    ohrb = P.buf("ohr")
    P.dma("sp", ohr[:, :], C.ohrev_dram, reads=[], writes=[ohrb])
    biasT = P.sb([128, 16, 256], BF16, "biasT")
    biasTb = P.buf("biasT")
    tab2p = P.sb([32, 128], F32, "tab2p")
    tab2pb = P.buf("tab2p")
    P.op("dve", lambda e: e.memset(tab2p[:, :], 0.0), reads=[], writes=[tab2pb])
    op_copy(P, "dve", tab2p[:, 0:16], tab2[:, :], [tab2b], [tab2pb])
    gi = 0
    for typ in range(2):
        for g in range(32):
            r = gi % 4
            gi += 1
            for tl in range(4):
                t = g * 4 + tl
                st = (383 - t) if typ == 0 else (255 - t)
                op_mm(P, ps[r][:, tl * 128:(tl + 1) * 128], ohr[:, st:st + 128], tab2p[:, :], True, True, [ohrb, tab2pb], [psb[r]])
            op_copy(P, "act" if g % 2 else "dve", biasT[:, :, typ * 128 + g * 4:typ * 128 + (g + 1) * 4],
                    ps[r][:, :].rearrange("p (t c) -> p t c", t=4)[:, :, 0:16].rearrange("p t h -> p h t"), [psb[r]], [biasTb])
    if ODD_STOP < 5:
        P.barrier()
        return
    mark3 = P.sb_off
    qT = [P.sb([128, S], BF16, "qT") for _ in range(1)]
    kT = [P.sb([128, S], BF16, "kT") for _ in range(1)]
    qTb, kTb = P.bufs_n(1, "qT"), P.bufs_n(1, "kT")
    qTm = [P.sb([128, S], BF16, "qTm") for _ in range(2)]
    qTmb = P.bufs_n(2, "qTm")
    vaug = P.sb([128, 32, 2, 65], BF16, "vaug2")
    vaugb = P.buf("vaug2")
    nstl = [P.sb([128, 32, 512], BF16, "nstl") for _ in range(2)]
    nstlb = P.bufs_n(2, "nstl")
    pts = [P.sb([128, 512], BF16, "pt") for _ in range(3)]
    ptb = P.bufs_n(3, "pt")
    fin = alloc_fin(P)
    P.op("pool", lambda e: e.memset(vaug[:, :, :, 64:65], 1.0), reads=[], writes=[vaugb])
    nload = 0
    for pr in (range(8) if "c" in OPARTS else []):
        P.dma("sp", qT[0][:, :], qT_dram[pr * 128:(pr + 1) * 128, :], reads=[qTb_d], writes=[qTb[0]])
        for hh in range(2):
            op_ts(P, "pool", qTm[hh][:, :], qT[0][:, :], C.cF[:, CF_HM + hh:CF_HM + hh + 1], None, ALU.mult, None,
                  [qTb[0], C.cFb], [qTmb[hh]])
        for sb_ in range(8):
            tsl = slice(sb_ * 512, (sb_ + 1) * 512)
            r = C.rot % 3
            C.rot += 1
            for lc in range(2):
                op_mm(P, ps[r][:, :], wukT[:, lc, pr, :], cT[:, lc, tsl], lc == 0, lc == 1, [wukTb] + cTb[sb_ * 4:sb_ * 4 + 4], [psb[r]])
            op_copy(P, "dve", kT[0][:, tsl], ps[r][:, :], [psb[r]], [kTb[0]])
        for tt in range(32):
            r = C.rot % 3
            C.rot += 1
            for lc in range(2):
                op_mm(P, ps[r][:, 0:128], cT[:, lc, tt * 128:(tt + 1) * 128],
                      wuv[:, lc, 2 * pr:2 * pr + 2, :], lc == 0, lc == 1, [wuvb, cTb[tt]], [psb[r]])
            op_copy(P, "act", vaug[:, tt, :, 0:64], ps[r][:, 0:128].rearrange("p (h d) -> p h d", h=2), [psb[r]], [vaugb])
        for qs in range(8):
            nk = 4 * qs + 4
            kb = nload % 2
            nload += 1
            if qs > 0:
                P.dma("sp", nstl[kb][:, 0:4 * qs, :], nsT_dram[0:4 * qs, :, qs * 512:(qs + 1) * 512].rearrange("k s t -> s k t"),
                      reads=[nsb_d], writes=[nstlb[kb]])
            for kc in range(4 * qs, nk):
                c0 = (kc - 4 * qs) * 128
                P.dma("sp", nstl[kb][:, kc, c0:512], nsT_dram[kc, :, kc * 128:(qs + 1) * 512], reads=[nsb_d], writes=[nstlb[kb]])
            for hh in range(2):
                h = 2 * pr + hh
                pp = slice(hh * 64, hh * 64 + 64)

                def out_cb(qs_, po, pob, h=h):
                    attn_finalize(P, C, po, pob, omixT_dram[h * 64:(h + 1) * 64, qs_ * 512:(qs_ + 1) * 512], omb, fin)
                attention_head(P, C, kT[0][:, :], kTb[0], qTm[hh][:, :], qTmb[hh], lambda kc, hh=hh: vaug[:, kc, hh, :], vaugb,
                               65, ptb, pts, out_cb,
                               negsel=lambda qs_, kb=kb: ((lambda kc: nstl[kb][:, kc, :]), nstlb[kb]),
                               nearb=(biasT[:, h, :], biasTb), qss=[qs])
    if ODD_STOP < 6:
        P.barrier()
        return
    P.barrier(keep=True)
    P.sb_off = SB_BASE + 16 * 1024
    if "4" in OPARTS:
        mixer_out_proj(P, C, src, xs, xbuf, omixT_dram, omb, w_out)
    P.barrier()


PHASES_ALL = ("f00", "even", "f01", "f10", "odd", "f11")


def build(phases=PHASES_ALL):
    nc = bass.Bass("TRN2", target_bir_lowering=False)
    dt = lambda name, shape, kind="ExternalInput", dtype=F32: nc.dram_tensor(name, list(shape), dtype, kind=kind).ap()
    x = dt("x", [S, D])
    norm_g = dt("norm_g", [2, 3, D])
    ffn_w_in = dt("ffn_w_in", [2, 2, D, 2 * DFF])
    ffn_w_out = dt("ffn_w_out", [2, 2, DFF, D])
    final_norm_g = dt("final_norm_g", [D])
    even_w_in = dt("even_w_in", [D, 3096])
    gla_w_gate = dt("gla_w_gate", [16, 256])
    gla_b_gate = dt("gla_b_gate", [256])
    gla_norm_g = dt("gla_norm_g", [128])
    fox_b_f = dt("fox_b_f", [8])
    even_w_out = dt("even_w_out", [D, D])
    odd_w_in = dt("odd_w_in", [D, 1864])
    mla_kv_norm_g = dt("mla_kv_norm_g", [256])
    mla_w_uk = dt("mla_w_uk", [16, 64, 256])
    mla_w_uv = dt("mla_w_uv", [16, 256, 64])
    odd_w_out = dt("odd_w_out", [D, D])
    t5_table = dt("t5_table", [32, 16])
    ident_bf = dt("ident_bf", [128, 128], dtype=BF16)
    ident_f = dt("ident_f", [128, 128])
    cF = dt("cF", [128, CF_N])
    negtri = dt("negtri", [128, 128], dtype=BF16)
    ohvec = dt("ohvec", [32, 512])
    out = dt("out", [S, D], kind="ExternalOutput")
    xs = dt("xs", [S, D], kind="Internal")
    omixT = dt("omixT", [D, S], kind="Internal", dtype=BF16)
    fsplit = dt("fsplit", [2, 8, 3, S], kind="Internal", dtype=BF16)
    nsT = dt("nsT", [32, 128, S], kind="Internal", dtype=BF16)
    qTd = dt("qTd", [D, S], kind="Internal", dtype=BF16)

    P = Prog(nc)
    C = Ctx()
    C.ident_dram = ident_bf
    C.identf_dram = ident_f
    C.cF_dram = cF
    C.negtri_dram = negtri
    C.ohrev_dram = ohvec
    C.ps = [nc.alloc_psum_tensor("ps%d" % i, [128, 512], F32) for i in range(6)]
    C.psb = [P.buf("ps%d" % i) for i in range(6)]
    C.psT = [nc.alloc_psum_tensor("psT%d" % i, [128, 1024], BF16) for i in range(2)]
    C.rot = C.rotp = C.rotf = C.roto = 0

    xbuf = P.buf("xs")
    C.outbuf = P.buf("out")
    cur = x
    for ph in phases:
        if ph.startswith("f") and ph != "fin":
            l, k = int(ph[1]), int(ph[2])
            last = (ph == phases[-1])
            ffn_phase_wrap(P, C, cur, xs, xbuf, norm_g[l, 2 * k, :], ffn_w_in[l, k], ffn_w_out[l, k],
                           final_norm_g if last else None, out)
            cur = xs
        elif ph == "even":
            even_phase(P, C, cur, xs, xbuf, norm_g[0, 1, :], even_w_in, gla_w_gate, gla_b_gate, gla_norm_g, fox_b_f,
                       even_w_out, omixT, fsplit)
            cur = xs
        elif ph == "odd":
            odd_phase(P, C, cur, xs, xbuf, norm_g[1, 1, :], odd_w_in, mla_kv_norm_g, mla_w_uk, mla_w_uv, odd_w_out,
                      t5_table, omixT, nsT, qTd)
            cur = xs
        elif ph == "fin":
            final_phase(P, C, xs, xbuf, final_norm_g, out)
        else:
            raise NotImplementedError(ph)
    P.emit()
    return nc


def final_phase(P, C, xs, xbuf, fg, out):
    alloc_common(P, C)
    C.fgB = P.sb([128, D], F32, "fgB")
    C.fgBb = P.buf("fgB")
    P.dma("sp", C.fgB[:, :], fg.partition_broadcast(128), reads=[], writes=[C.fgBb])
    C = copy.copy(C)
    xt = P.sb([128, 4, D], F32, "xt")
    xtb = P.bufs_n(4, "xt")
    for tt in range(32):
        i = tt % 4
        P.dma("sp", xt[:, i, :], xs[tt * 128:(tt + 1) * 128, :], reads=[xbuf], writes=[xtb[i]])
        final_norm_tile(P, C, xt, xtb, i, tt, fg, out)
    P.barrier()


def ffn_phase_wrap(P, C, src, dst, xbuf, g, w_in, w_out, fg, out):
    if fg is not None:
        C.fgB = P.sb([128, D], F32, "fgB")
        C.fgBb = P.buf("fgB")
        P.dma("sp", C.fgB[:, :], fg.partition_broadcast(128), reads=[], writes=[C.fgBb])
    ffn_phase(P, C, src, dst, xbuf, g, w_in, w_out, fg, out)


_CACHE = {}


def kernel(**inputs):
    import ml_dtypes
    phases = inputs.pop("_phases", PHASES_ALL)
    key = tuple(phases)
    if key not in _CACHE:
        _CACHE[key] = build(phases)
    nc = _CACHE[key]
    x = np.ascontiguousarray(inputs["x"], dtype=np.float32)
    shared = {
        "norm_g": np.ascontiguousarray(inputs["norm_g"], dtype=np.float32),
        "ffn_w_in": np.ascontiguousarray(inputs["ffn_w_in"], dtype=np.float32),
        "ffn_w_out": np.ascontiguousarray(inputs["ffn_w_out"], dtype=np.float32),
        "final_norm_g": np.ascontiguousarray(inputs["final_norm_g"], dtype=np.float32),
        "ident_bf": np.eye(128, dtype=np.float32).astype(ml_dtypes.bfloat16),
        "ident_f": np.eye(128, dtype=np.float32),
        "even_w_in": np.ascontiguousarray(inputs["even_w_in"][0], dtype=np.float32),
        "gla_w_gate": np.ascontiguousarray(inputs["gla_w_gate"][0], dtype=np.float32),
        "gla_b_gate": np.ascontiguousarray(inputs["gla_b_gate"][0], dtype=np.float32),
        "gla_norm_g": np.ascontiguousarray(inputs["gla_norm_g"][0], dtype=np.float32),
        "fox_b_f": np.ascontiguousarray(inputs["fox_b_f"][0], dtype=np.float32),
        "even_w_out": np.ascontiguousarray(inputs["even_w_out"][0], dtype=np.float32),
        "odd_w_in": np.ascontiguousarray(inputs["odd_w_in"][0], dtype=np.float32),
        "mla_kv_norm_g": np.ascontiguousarray(inputs["mla_kv_norm_g"][0], dtype=np.float32),
        "mla_w_uk": np.ascontiguousarray(inputs["mla_w_uk"][0], dtype=np.float32),
        "mla_w_uv": np.ascontiguousarray(inputs["mla_w_uv"][0], dtype=np.float32),
        "odd_w_out": np.ascontiguousarray(inputs["odd_w_out"][0], dtype=np.float32),
        "t5_table": np.ascontiguousarray(inputs["t5_table"], dtype=np.float32),
    }
    cF_, negtri_ = host_consts()
    shared["cF"] = cF_
    shared["negtri"] = negtri_
    shared["ohvec"] = np.ascontiguousarray(host_ohvec()[:, ::-1])
    in_maps = []
    for c in range(8):
        m = dict(shared)
        m["x"] = x[c]
        in_maps.append(m)
    res = run_bass_kernel_spmd(nc, in_maps, core_ids=list(range(8)))
    return np.stack([np.asarray(r["out"], dtype=np.float32) for r in res.results], axis=0)
```

```python
import copy
import os
import numpy as np

PARTS = os.environ.get("EVEN_PARTS", "gvshp")
GLA_STOP = float(os.environ.get("GLA_STOP", "99"))
OPARTS = os.environ.get("ODD_PARTS", "12abc4")
ODD_STOP = float(os.environ.get("ODD_STOP", "99"))
import concourse.bass as bass
import concourse.mybir as mybir
from concourse.bass_utils import run_bass_kernel_spmd

F32 = mybir.dt.float32
BF16 = mybir.dt.bfloat16
AF = mybir.ActivationFunctionType
ALU = mybir.AluOpType
AX = mybir.AxisListType

S = 4096
D = 1024
DFF = 2816
NT = S // 128
EPS = 1e-6
SB_BASE = 16384 + 128
ARENA_BYTES = 204 * 1024


class Buf:
    __slots__ = ("name", "w", "r", "dsem")

    def __init__(self, name=""):
        self.name = name
        self.w = {}
        self.r = {}
        self.dsem = None


class DSem:
    __slots__ = ("sem", "val", "key", "qt")

    def __init__(self, sem, key, qt):
        self.sem = sem
        self.val = 0
        self.key = key
        self.qt = qt


class Prog:
    ENGS = ("pe", "act", "dve", "pool", "sp")

    def __init__(self, nc, n_dsem=90):
        self.nc = nc
        self.lists = {e: [] for e in self.ENGS}
        self.cnt = {e: 0 for e in self.ENGS}
        self.seen = {e: {} for e in self.ENGS}
        self.sem = {e: nc.alloc_semaphore("s_" + e) for e in self.ENGS}
        self.dfree = {"hw": [DSem(nc.alloc_semaphore("d%d" % i), "d%d" % i, "hw") for i in range(n_dsem - 24)],
                      "sw": [DSem(nc.alloc_semaphore("w%d" % i), "w%d" % i, "sw") for i in range(24)]}
        self.dused = []
        self.bufs = []
        self.sb_off = SB_BASE
        self.uid = 0
        self.arena = nc.alloc_sbuf_tensor("arena", [128, ARENA_BYTES // 4], F32)

    def buf(self, name=""):
        b = Buf(name)
        self.bufs.append(b)
        return b

    def bufs_n(self, n, name=""):
        return [self.buf(name + str(i)) for i in range(n)]

    def sb(self, shape, dtype, name="t"):
        esz = 2 if dtype == BF16 else 4
        n = 1
        for s_ in shape[1:]:
            n *= s_
        nwords = ((n * esz + 63) // 64 * 64) // 4
        w0 = (self.sb_off - SB_BASE) // 4
        self.sb_off += nwords * 4
        assert self.sb_off <= SB_BASE + ARENA_BYTES, ("SBUF overflow", self.sb_off)
        v = self.arena[0:shape[0], w0:w0 + nwords]
        if dtype == BF16:
            v = v.bitcast(BF16)
        v = v[:, 0:n]
        if len(shape) == 3:
            v = v.rearrange("p (a b) -> p a b", a=shape[1])
        elif len(shape) == 4:
            v = v.rearrange("p (a b c) -> p a b c", a=shape[1], b=shape[2])
        return v

    def _deps(self, reads, writes):
        deps = {}
        for b in reads:
            for k, v in b.w.items():
                if k not in deps or deps[k][1] < v[1]:
                    deps[k] = v
        for b in writes:
            for dd in (b.w, b.r):
                for k, v in dd.items():
                    if k not in deps or deps[k][1] < v[1]:
                        deps[k] = v
        return deps

    def _filter(self, eng, deps):
        waits = []
        for k, (s_, c) in deps.items():
            if k == eng:
                if eng == "pe":
                    continue
                if c <= self.cnt[eng] - 3:
                    continue
            if self.seen[eng].get(k, 0) >= c:
                continue
            self.seen[eng][k] = c
            waits.append((s_, c))
        return waits

    def op(self, eng, fn, reads=(), writes=()):
        waits = self._filter(eng, self._deps(reads, writes))
        self.cnt[eng] += 1
        tok = (self.sem[eng], self.cnt[eng])
        self.lists[eng].append(("op", waits, fn))
        for b in reads:
            b.r[eng] = tok
        for b in writes:
            b.w[eng] = tok
        return tok

    def dma(self, q, out_ap, in_ap, reads=(), writes=(), **kw):
        waits = self._filter(q, self._deps(reads, writes))
        b0 = writes[0]
        qt = "sw" if q == "pool" else "hw"
        if b0.dsem is None:
            b0.dsem = self.dfree[qt].pop()
            self.dused.append(b0.dsem)
        ds = b0.dsem
        assert ds.qt == qt, ("buffer DMA-written from both SW and HW DGE", b0.name)
        ds.val += 16
        tok = (ds.sem, ds.val)
        self.lists[q].append(("dma", waits, out_ap, in_ap, ds.sem, kw))
        for b in reads:
            b.r[ds.key] = tok
        for b in writes:
            b.w[ds.key] = tok
        return tok

    def barrier(self, keep=False):
        for e in self.ENGS:
            waits = []
            for e2 in self.ENGS:
                if e2 != e and self.cnt[e2] > self.seen[e].get(e2, 0):
                    waits.append((self.sem[e2], self.cnt[e2]))
                    self.seen[e][e2] = self.cnt[e2]
            for ds in self.dused:
                waits.append((ds.sem, ds.val))
            self.lists[e].append(("wait", waits))
        if keep:
            return
        for ds in self.dused:
            self.dfree[ds.qt].append(ds)
        self.dused = []
        for b in self.bufs:
            b.w = {}
            b.r = {}
            b.dsem = None
        self.sb_off = SB_BASE

    def emit(self):
        nc = self.nc
        with nc.Block() as block:
            def mk(name):
                def body(e):
                    own = self.sem[name]
                    for item in self.lists[name]:
                        for (s_, c) in item[1]:
                            e.wait_ge(s_, c)
                        if item[0] == "op":
                            item[2](e).then_inc(own, 1)
                        elif item[0] == "dma":
                            e.dma_start(out=item[2], in_=item[3], **item[5]).then_inc(item[4], 16)
                return body
            block.tensor(mk("pe"))
            block.scalar(mk("act"))
            block.vector(mk("dve"))
            block.gpsimd(mk("pool"))
            block.sync(mk("sp"))


class Ctx:
    pass


def load_norm_transpose(P, C, src, srcbuf, gB, gBb, xt, xtb, hT, hTb, tiles, keep_x=True):
    nc = P.nc
    n = len(tiles)
    for i, tt in enumerate(tiles):
        P.dma("sp", xt[:, i, :], src[tt * 128:(tt + 1) * 128, :], reads=[srcbuf], writes=[xtb[i]])
    for i, tt in enumerate(tiles):
        k = i % 2
        junk, junkb = C.junk[k], C.junkb[k]
        ss, ssb = C.ss, C.ssb[k]
        P.op("act", lambda e, i=i, k=k: e.activation(out=C.junk[k][:, :], in_=xt[:, i, :], func=AF.Square,
                                                     accum_out=C.ss[:, k:k + 1]),
             reads=[xtb[i]], writes=[junkb, ssb])
        P.op("dve", lambda e, k=k: e.tensor_scalar(out=C.ss2[:, k:k + 1], in0=C.ss[:, k:k + 1], scalar1=1.0 / D,
                                                   scalar2=EPS, op0=ALU.mult, op1=ALU.add),
             reads=[ssb], writes=[C.ss2b[k]])
        P.op("act", lambda e, k=k: e.activation(out=C.ss3[:, k:k + 1], in_=C.ss2[:, k:k + 1], func=AF.Sqrt),
             reads=[C.ss2b[k]], writes=[C.ss3b[k]])
        P.op("dve", lambda e, k=k: e.reciprocal(out=C.rstd[:, k:k + 1], in_=C.ss3[:, k:k + 1]),
             reads=[C.ss3b[k]], writes=[C.rstdb[k]])
        P.op("dve", lambda e, i=i, k=k: e.scalar_tensor_tensor(out=C.hn[k][:, :], in0=xt[:, i, :],
                                                               scalar=C.rstd[:, k:k + 1], in1=gB[:, :],
                                                               op0=ALU.mult, op1=ALU.mult),
             reads=[xtb[i], C.rstdb[k], gBb], writes=[C.hnb[k]])
        for dc in range(8):
            P.op("pe", lambda e, dc=dc, k=k: e.transpose(out=C.psT[k][:, dc * 128:(dc + 1) * 128],
                                                         in_=C.hn[k][:, dc * 128:(dc + 1) * 128],
                                                         identity=C.ident[:, :]),
                 reads=[C.hnb[k], C.identb], writes=[C.psTb[k]])
        eng = "act" if (i % 2 == 0) else "dve"
        if eng == "act":
            P.op("act", lambda e, i=i, k=k: e.copy(out=hT[:, :, i * 128:(i + 1) * 128],
                                                   in_=C.psT[k][:, :].rearrange("p (c t) -> p c t", c=8)),
                 reads=[C.psTb[k]], writes=[hTb[i]])
        else:
            P.op("dve", lambda e, i=i, k=k: e.tensor_copy(out=hT[:, :, i * 128:(i + 1) * 128],
                                                          in_=C.psT[k][:, :].rearrange("p (c t) -> p c t", c=8)),
                 reads=[C.psTb[k]], writes=[hTb[i]])


def alloc_common(P, C):
    C.ident = P.sb([128, 128], BF16, "ident")
    C.identb = P.buf("ident")
    C.identf = P.sb([128, 128], F32, "identf")
    C.junk = [P.sb([128, 1024], BF16, "junk") for _ in range(2)]
    C.junkb = P.bufs_n(2, "junk")
    C.ss = P.sb([128, 2], F32, "ss")
    C.ss2 = P.sb([128, 2], F32, "ss2")
    C.ss3 = P.sb([128, 2], F32, "ss3")
    C.rstd = P.sb([128, 2], F32, "rstd")
    C.ssb = P.bufs_n(2, "ss")
    C.ss2b = P.bufs_n(2, "ss2")
    C.ss3b = P.bufs_n(2, "ss3")
    C.rstdb = P.bufs_n(2, "rstd")
    C.hn = [P.sb([128, 1024], BF16, "hn") for _ in range(2)]
    C.hnb = P.bufs_n(2, "hn")
    C.psTb = P.bufs_n(2, "psT")
    P.dma("sp", C.ident[:, :], C.ident_dram[:, :], reads=[], writes=[C.identb])
    P.dma("sp", C.identf[:, :], C.identf_dram[:, :], reads=[], writes=[C.identb])


def op_act(P, out, in_, func, reads, writes, **kw):
    return P.op("act", lambda e: e.activation(out=out, in_=in_, func=func, **kw), reads=reads, writes=writes)


def op_mm(P, out, lhsT, rhs, start, stop, reads, writes):
    return P.op("pe", lambda e: e.matmul(out, lhsT, rhs, start=start, stop=stop), reads=reads, writes=writes)


def op_tt(P, eng, out, in0, in1, op, reads, writes):
    return P.op(eng, lambda e: e.tensor_tensor(out=out, in0=in0, in1=in1, op=op), reads=reads, writes=writes)


def op_ts(P, eng, out, in0, s1, s2, op0, op1, reads, writes, **kw):
    if op1 is None:
        return P.op(eng, lambda e: e.tensor_scalar(out=out, in0=in0, scalar1=s1, scalar2=None, op0=op0, **kw),
                    reads=reads, writes=writes)
    return P.op(eng, lambda e: e.tensor_scalar(out=out, in0=in0, scalar1=s1, scalar2=s2, op0=op0, op1=op1, **kw),
                reads=reads, writes=writes)


def op_stt(P, out, in0, scalar, in1, op0, op1, reads, writes):
    return P.op("dve", lambda e: e.scalar_tensor_tensor(out=out, in0=in0, scalar=scalar, in1=in1, op0=op0, op1=op1),
                reads=reads, writes=writes)


def op_copy(P, eng, out, in_, reads, writes):
    if eng == "act":
        return P.op("act", lambda e: e.copy(out=out, in_=in_), reads=reads, writes=writes)
    return P.op(eng, lambda e: e.tensor_copy(out=out, in_=in_), reads=reads, writes=writes)


def ffn_phase(P, C, src, dst, xbuf, g_dram, w_in, w_out, final_g=None, out_dram=None):
    nc = P.nc
    alloc_common(P, C)
    C = copy.copy(C)
    TP = 1024
    NP = S // TP
    gB = P.sb([128, D], F32, "gB")
    gBb = P.buf("gB")
    P.dma("sp", gB[:, :], g_dram.partition_broadcast(128), reads=[], writes=[gBb])
    xt = P.sb([128, 8, D], F32, "xt")
    xtb = P.bufs_n(8, "xt")
    hT = P.sb([128, 8, TP], BF16, "hT")
    hTb = P.bufs_n(8, "hT")
    gT = P.sb([128, 22, TP], BF16, "gT")
    gTb = [[P.buf("gT") for _ in range(2)] for _ in range(22)]
    NWB = 3
    winA = [P.sb([128, 8, 256], BF16, "winA") for _ in range(NWB)]
    winB = [P.sb([128, 8, 256], BF16, "winB") for _ in range(NWB)]
    winAb = P.bufs_n(NWB, "winA")
    winBb = P.bufs_n(NWB, "winB")
    wout = P.sb([128, 22, D], BF16, "wout")
    woutb = P.bufs_n(2, "wout")
    sg = [P.sb([128, 512], F32, "sg") for _ in range(2)]
    sgb = P.bufs_n(2, "sg")
    w_out_v = w_out.rearrange("(fc p) n -> p fc n", p=128)
    for h in range(2):
        P.dma("pool", wout[:, h * 11:(h + 1) * 11, :], w_out_v[:, h * 11:(h + 1) * 11, :], reads=[], writes=[woutb[h]])
    w_in_v = w_in.rearrange("(dc p) n -> p dc n", p=128)
    blk = 0
    for p in range(NP):
        tiles = list(range(p * 8, p * 8 + 8))
        load_norm_transpose(P, C, src, xbuf, gB, gBb, xt, xtb, hT, hTb, tiles)
        for jb in range(11):
            wb = blk % NWB
            blk += 1
            P.dma("pool", winA[wb][:, :, :], w_in_v[:, :, jb * 256:(jb + 1) * 256], reads=[], writes=[winAb[wb]])
            P.dma("pool", winB[wb][:, :, :], w_in_v[:, :, DFF + jb * 256:DFF + (jb + 1) * 256], reads=[],
                  writes=[winBb[wb]])
            for jj in range(2):
                j = jb * 2 + jj
                for half in range(2):
                    pa, pab = C.ps[half], C.psb[half]
                    pb, pbb = C.ps[2 + half], C.psb[2 + half]
                    for dc in range(8):
                        P.op("pe", lambda e, pa=pa, wb=wb, dc=dc, jj=jj, half=half: e.matmul(
                            pa[:, :], winA[wb][:, dc, jj * 128:(jj + 1) * 128], hT[:, dc, half * 512:(half + 1) * 512],
                            start=(dc == 0), stop=(dc == 7)),
                            reads=[winAb[wb]] + hTb[half * 4:half * 4 + 4], writes=[pab])
                    for dc in range(8):
                        P.op("pe", lambda e, pb=pb, wb=wb, dc=dc, jj=jj, half=half: e.matmul(
                            pb[:, :], winB[wb][:, dc, jj * 128:(jj + 1) * 128], hT[:, dc, half * 512:(half + 1) * 512],
                            start=(dc == 0), stop=(dc == 7)),
                            reads=[winBb[wb]] + hTb[half * 4:half * 4 + 4], writes=[pbb])
                    P.op("act", lambda e, pa=pa, half=half: e.activation(out=sg[half][:, :], in_=pa[:, :], func=AF.Silu),
                         reads=[pab], writes=[sgb[half]])
                    P.op("dve", lambda e, pb=pb, half=half, j=j: e.tensor_tensor(
                        out=gT[:, j, half * 512:(half + 1) * 512], in0=sg[half][:, :], in1=pb[:, :], op=ALU.mult),
                        reads=[sgb[half], pbb], writes=[gTb[j][half]])
        for i in range(8):
            tt = tiles[i]
            for n in range(2):
                py, pyb = C.ps[4 + n], C.psb[4 + n]
                for fc in range(22):
                    P.op("pe", lambda e, py=py, fc=fc, i=i, n=n: e.matmul(
                        py[:, :], gT[:, fc, i * 128:(i + 1) * 128], wout[:, fc, n * 512:(n + 1) * 512],
                        start=(fc == 0), stop=(fc == 21)),
                        reads=[gTb[fc][i // 4], woutb[fc // 11]], writes=[pyb])
                P.op("dve", lambda e, py=py, i=i, n=n: e.scalar_tensor_tensor(
                    out=xt[:, i, n * 512:(n + 1) * 512], in0=py[:, :], scalar=0.5, in1=xt[:, i, n * 512:(n + 1) * 512],
                    op0=ALU.mult, op1=ALU.add),
                    reads=[pyb, xtb[i]], writes=[xtb[i]])
            if final_g is None:
                P.dma("sp", dst[tt * 128:(tt + 1) * 128, :], xt[:, i, :], reads=[xtb[i]], writes=[xbuf])
            else:
                final_norm_tile(P, C, xt, xtb, i, tt, final_g, out_dram)
    P.barrier()


def final_norm_tile(P, C, xt, xtb, i, tt, fg, out_dram):
    k = i % 2
    P.op("act", lambda e, i=i, k=k: e.activation(out=C.junk[k][:, :], in_=xt[:, i, :], func=AF.Square,
                                                 accum_out=C.ss[:, k:k + 1]),
         reads=[xtb[i]], writes=[C.junkb[k], C.ssb[k]])
    P.op("dve", lambda e, k=k: e.tensor_scalar(out=C.ss2[:, k:k + 1], in0=C.ss[:, k:k + 1], scalar1=1.0 / D,
                                               scalar2=EPS, op0=ALU.mult, op1=ALU.add),
         reads=[C.ssb[k]], writes=[C.ss2b[k]])
    P.op("act", lambda e, k=k: e.activation(out=C.ss3[:, k:k + 1], in_=C.ss2[:, k:k + 1], func=AF.Sqrt),
         reads=[C.ss2b[k]], writes=[C.ss3b[k]])
    P.op("dve", lambda e, k=k: e.reciprocal(out=C.rstd[:, k:k + 1], in_=C.ss3[:, k:k + 1]),
         reads=[C.ss3b[k]], writes=[C.rstdb[k]])
    P.op("dve", lambda e, i=i, k=k: e.scalar_tensor_tensor(out=xt[:, i, :], in0=xt[:, i, :],
                                                           scalar=C.rstd[:, k:k + 1], in1=C.fgB[:, :],
                                                           op0=ALU.mult, op1=ALU.mult),
         reads=[xtb[i], C.rstdb[k], C.fgBb], writes=[xtb[i]])
    P.dma("sp", out_dram[tt * 128:(tt + 1) * 128, :], xt[:, i, :], reads=[xtb[i]], writes=[C.outbuf])


CF_TRI, CF_REV, CF_MASK4, CF_ONES, CF_POW, CF_HM, CF_E, CF_N = 0, 128, 256, 768, 896, 928, 930, 994


def host_ohvec():
    oh = np.zeros((32, 512), np.float32)
    for idx in range(512):
        d = idx - 128
        if d < 0:
            continue
        if d < 16:
            b = d
        else:
            v = np.float32(np.log(np.float32(max(d, 1)) / np.float32(16.0))) / np.float32(np.log(128.0 / 16.0)) * np.float32(16.0)
            b = min(16 + int(v), 31)
        oh[b, idx] = 1.0
    return oh


def host_consts():
    import ml_dtypes
    j = np.arange(128)[:, None]
    i = np.arange(128)[None, :]
    cF = np.zeros((128, CF_N), np.float32)
    cF[:, CF_TRI:CF_TRI + 128] = (j <= i)
    cF[:, CF_REV:CF_REV + 128] = (j > i)
    cF[:, CF_MASK4:CF_MASK4 + 512] = np.tile((j <= i).astype(np.float32), (1, 4))
    cF[:, CF_ONES:CF_ONES + 128] = 1.0
    for i_ in range(NBIS):
        cF[:, CF_POW + i_] = 2.0 ** -(i_ + 1)
    cF[:, CF_POW + NBIS] = 2.0 ** -(NBIS + 1)
    cF[0:64, CF_HM] = 1.0
    cF[64:128, CF_HM + 1] = 1.0
    cF[64, CF_E:CF_E + 64] = 1.0
    negtri = np.where(j > i, -30000.0, 0.0).astype(np.float32).astype(ml_dtypes.bfloat16)
    return cF, negtri


def load_consts(P, C):
    C.cF = P.sb([128, CF_N], F32, "cF")
    C.cFb = P.buf("cF")
    P.dma("sp", C.cF[:, :], C.cF_dram[:, :], reads=[], writes=[C.cFb])
    C.negtri = P.sb([128, 128], BF16, "negtri")
    C.negtrib = P.buf("negtri")
    P.dma("sp", C.negtri[:, :], C.negtri_dram[:, :], reads=[], writes=[C.negtrib])


def build_hT_full(P, C, src, srcbuf, g_dram):
    hT = P.sb([128, 8, S], BF16, "hT")
    hTb = P.bufs_n(32, "hT")
    C.mark = P.sb_off
    gB = P.sb([128, D], F32, "gB")
    gBb = P.buf("gB")
    P.dma("sp", gB[:, :], g_dram.partition_broadcast(128), reads=[], writes=[gBb])
    xt = P.sb([128, 4, D], F32, "xt")
    xtb = P.bufs_n(4, "xt")
    for grp in range(8):
        load_norm_transpose(P, C, src, srcbuf, gB, gBb, xt, xtb, hT[:, :, grp * 512:(grp + 1) * 512],
                            hTb[grp * 4:grp * 4 + 4], list(range(grp * 4, grp * 4 + 4)))
    P.barrier(keep=True)
    P.sb_off = C.mark
    return hT, hTb


def attention_head(P, C, kA, kAb, qA, qAb, vfn, vb, M, ptb, pts, out_cb, negsel=None, nearb=None, fox=False,
                   qss=range(8)):
    for qs in qss:
        po, pob = C.ps[4 + C.roto % 2], C.psb[4 + C.roto % 2]
        C.roto += 1
        nk = 4 * qs + 4
        if negsel is not None:
            nsfn, nsb = negsel(qs)
        def issue_qk(kc):
            t_lo = max(qs * 512, kc * 128)
            N = (qs + 1) * 512 - t_lo
            c0 = t_lo - qs * 512
            r = C.rot % 3
            C.rot += 1
            ps_, psb_ = C.ps[r], C.psb[r]
            extra = []
            if negsel is not None:
                extra.append((C.ident[:, :], nsfn(kc)[:, c0:c0 + N], 0, N, [C.identb, nsb]))
            diag_in = kc * 128 >= qs * 512
            if fox and diag_in:
                extra.append((C.ident[:, :], C.negtri[:, :], 0, 128, [C.identb, C.negtrib]))
            if nearb is not None:
                nb_ap, nb_b = nearb
                if diag_in and kc % 4 != 3:
                    extra.append((C.ident[:, :], nb_ap[:, 0:256], 0, 256, [C.identb, nb_b]))
                elif diag_in:
                    extra.append((C.ident[:, :], nb_ap[:, 0:128], 0, 128, [C.identb, nb_b]))
                elif kc == 4 * qs - 1:
                    extra.append((C.ident[:, :], nb_ap[:, 128:256], 0, 128, [C.identb, nb_b]))
            op_mm(P, ps_[:, 0:N], kA[:, kc * 128:(kc + 1) * 128], qA[:, t_lo:t_lo + N], True, len(extra) == 0,
                  [kAb, qAb], [psb_])
            for ei, (l_, r_, a, n_, rb) in enumerate(extra):
                op_mm(P, ps_[:, a:a + n_], l_, r_, False, ei == len(extra) - 1, rb, [psb_])
            return ps_, psb_, N, c0

        def issue_rest(kc, info):
            ps_, psb_, N, c0 = info
            pr = C.rotp % len(pts)
            C.rotp += 1
            op_act(P, pts[pr][:, 0:N], ps_[:, 0:N], AF.Exp, [psb_], [ptb[pr]])
            op_mm(P, po[0:M, c0:c0 + N], vfn(kc), pts[pr][:, 0:N], kc == 0, kc == nk - 1, [vb, ptb[pr]], [pob])

        pend = [issue_qk(0)]
        if nk > 1:
            pend.append(issue_qk(1))
        for kc in range(nk):
            if kc + 2 < nk:
                pend.append(issue_qk(kc + 2))
            issue_rest(kc, pend.pop(0))
        out_cb(qs, po, pob)


def attn_finalize(P, C, po, pob, dst_ap, dstbuf, fin):
    k = C.rotf % 2
    C.rotf += 1
    num, numb, bcS, bcSb, oS, oSb = fin["num"][k], fin["numb"][k], fin["bcS"][k], fin["bcSb"][k], fin["oS"][k], fin["oSb"][k]
    op_copy(P, "act", num[0:65, :], po[0:65, :], [pob], [numb])
    pb, pbb = C.ps[3], C.psb[3]
    op_mm(P, pb[0:64, :], C.cF[0:65, CF_E:CF_E + 64], num[0:65, :], True, True, [C.cFb, numb], [pbb])
    P.op("dve", lambda e: e.reciprocal(out=bcS[0:64, :], in_=pb[0:64, :]), reads=[pbb], writes=[bcSb])
    op_tt(P, "dve", oS[0:64, :], num[0:64, :], bcS[0:64, :], ALU.mult, [numb, bcSb], [oSb])
    P.dma("sp", dst_ap, oS[0:64, :], reads=[oSb], writes=[dstbuf])


def alloc_fin(P):
    fin = {}
    fin["num"] = [P.sb([65, 512], F32, "num") for _ in range(2)]
    fin["numb"] = P.bufs_n(2, "num")
    fin["bcS"] = [P.sb([64, 512], F32, "bcS") for _ in range(2)]
    fin["bcSb"] = P.bufs_n(2, "bcS")
    fin["oS"] = [P.sb([64, 512], BF16, "oS") for _ in range(2)]
    fin["oSb"] = P.bufs_n(2, "oS")
    return fin


def even_phase(P, C, src, xs, xbuf, g_dram, w_in, w_gate, b_gate, gla_g, fox_b, w_out, omixT_dram, fsplit_dram):
    alloc_common(P, C)
    load_consts(P, C)
    C = copy.copy(C)
    omb = P.buf("omixT_dram")
    fsb = P.buf("fsplit_dram")
    hT, hTb = build_hT_full(P, C, src, xbuf, g_dram)
    mark = C.mark
    w_in_v = w_in.rearrange("(dc p) n -> p dc n", p=128)
    omix_v = omixT_dram.rearrange("(c p) t -> p c t", p=128)

    wg = P.sb([128, 8, 1552], BF16, "wg")
    wgb = P.buf("wg")
    P.dma("pool", wg[:, :, :], w_in_v[:, :, 0:1552], reads=[], writes=[wgb])
    wgate = P.sb([16, 256], F32, "wgate")
    bgate = P.sb([1, 256], F32, "bgate")
    gwb = P.buf("gw")
    P.dma("sp", wgate[:, :], w_gate, reads=[], writes=[gwb])
    P.dma("sp", bgate[:, :], b_gate.rearrange("(o n) -> o n", o=1), reads=[], writes=[gwb])
    gnB = P.sb([128, 128], F32, "gnB")
    P.dma("sp", gnB[:, :], gla_g.partition_broadcast(128), reads=[], writes=[gwb])
    qTs = P.sb([128, 2, 512], F32, "qTs")
    kTs = P.sb([128, 2, 512], F32, "kTs")
    qTsb, kTsb = P.buf("qTs"), P.buf("kTs")
    gkl = P.sb([16, 512], F32, "gkl")
    gklb = P.buf("gkl")
    vtok = P.sb([128, 512], BF16, "vtok")
    vtokb = P.buf("vtok")
    ktok = P.sb([128, 256], F32, "ktok")
    ktokb = P.buf("ktok")
    sgo = P.sb([128, 512], F32, "sgo")
    sgob = P.buf("sgo")
    ez = P.sb([128, 256], F32, "ez")
    ezb = P.buf("ez")
    Lt = P.sb([128, 256], F32, "Lt")
    Ltb = P.buf("Lt")
    e1 = P.sb([128, 256], F32, "e1")
    e2 = P.sb([128, 256], F32, "e2")
    e3 = P.sb([128, 256], F32, "e3")
    e1b, e2b, e3b = P.buf("e1"), P.buf("e2"), P.buf("e3")
    qdT = P.sb([128, 2, 2, 128], BF16, "qdT")
    kdT = P.sb([128, 2, 2, 128], BF16, "kdT")
    qdTb, kdTb = P.buf("qdT"), P.buf("kdT")
    kte = P.sb([128, 256], BF16, "kte")
    kteb = P.buf("kte")
    AT = P.sb([128, 512], BF16, "AT")
    ATb = P.buf("AT")
    St = P.sb([128, 2, 128], F32, "St")
    Sbf = P.sb([128, 2, 128], BF16, "Sbf")
    Stb = P.bufs_n(2, "St")
    Sbfb = P.bufs_n(2, "Sbf")
    dec = P.sb([128, 2], F32, "dec")
    decb = P.buf("dec")
    osq = P.sb([128, 512], F32, "osq")
    osqb = P.buf("osq")
    sm = P.sb([128, 16], F32, "sm")
    smb = P.bufs_n(4, "sm")
    t1 = P.sb([128, 512], F32, "t1")
    t1b = P.buf("t1")
    og = P.sb([128, 512], BF16, "og")
    ogb = P.buf("og")
    ogT = [P.sb([128, 4, 128], BF16, "ogT") for _ in range(2)]
    ogTb = P.bufs_n(2, "ogT")
    P.op("dve", lambda e: e.memset(St[:, :, :], 0.0), reads=[], writes=Stb)
    ps = C.ps
    psb = C.psb
    for sb_ in (range(8) if "g" in PARTS else []):
        tsl = slice(sb_ * 512, (sb_ + 1) * 512)
        hb = hTb[sb_ * 4:sb_ * 4 + 4]
        for c in range(2):
            for dc in range(8):
                op_mm(P, ps[0][:, :], wg[:, dc, c * 128:(c + 1) * 128], hT[:, dc, tsl], dc == 0, dc == 7, [wgb] + hb, [psb[0]])
            op_act(P, qTs[:, c, :], ps[0][:, :], AF.Copy, [psb[0]], [qTsb], scale=0.125)
            for dc in range(8):
                op_mm(P, ps[1][:, :], wg[:, dc, 256 + c * 128:256 + (c + 1) * 128], hT[:, dc, tsl], dc == 0, dc == 7, [wgb] + hb, [psb[1]])
            op_copy(P, "dve", kTs[:, c, :], ps[1][:, :], [psb[1]], [kTsb])
        for dc in range(8):
            op_mm(P, ps[2][0:16, :], wg[:, dc, 1536:1552], hT[:, dc, tsl], dc == 0, dc == 7, [wgb] + hb, [psb[2]])
        op_copy(P, "act", gkl[:, :], ps[2][0:16, :], [psb[2]], [gklb])
        for ch in range(4):
            tt = sb_ * 4 + ch
            ttl = slice(tt * 128, (tt + 1) * 128)
            csl = slice(ch * 128, (ch + 1) * 128)
            if GLA_STOP < 4:
                continue
            for dc in range(8):
                op_mm(P, ps[0][:, 0:256], hT[:, dc, ttl], wg[:, dc, 256:512], dc == 0, dc == 7, [wgb, hTb[tt]], [psb[0]])
            for dc in range(8):
                op_mm(P, ps[1][:, :], hT[:, dc, ttl], wg[:, dc, 512:1024], dc == 0, dc == 7, [wgb, hTb[tt]], [psb[1]])
            op_copy(P, "act", vtok[:, :], ps[1][:, :], [psb[1]], [vtokb])
            op_copy(P, "act", ktok[:, :], ps[0][:, 0:256], [psb[0]], [ktokb])
            for dc in range(8):
                op_mm(P, ps[2][:, :], hT[:, dc, ttl], wg[:, dc, 1024:1536], dc == 0, dc == 7, [wgb, hTb[tt]], [psb[2]])
            op_act(P, sgo[:, :], ps[2][:, :], AF.Silu, [psb[2]], [sgob])
            if GLA_STOP < 5:
                continue
            op_mm(P, ps[3][:, 0:256], gkl[0:16, csl], wgate[0:16, :], True, False, [gklb, gwb], [psb[3]])
            op_mm(P, ps[3][:, 0:256], C.cF[0:1, CF_ONES:CF_ONES + 128], bgate[0:1, :], False, True, [C.cFb, gwb], [psb[3]])
            op_act(P, ez[:, :], ps[3][:, 0:256], AF.Exp, [psb[3]], [ezb], scale=-1.0)
            op_act(P, Lt[:, :], ez[:, :], AF.Ln, [ezb], [Ltb], bias=1.0)
            if GLA_STOP < 6:
                continue
            for c in range(2):
                op_mm(P, ps[3][:, 256 + c * 128:256 + (c + 1) * 128], Lt[:, c * 128:(c + 1) * 128],
                      C.cF[:, CF_TRI:CF_TRI + 128], True, True, [Ltb, C.cFb], [psb[3]])
            if GLA_STOP < 6.2:
                continue
            op_act(P, e1[:, :], ps[3][:, 256:512], AF.Exp, [psb[3]], [e1b], scale=-1.0 / 16)
            op_act(P, e2[:, :], ps[3][:, 256:512], AF.Exp, [psb[3]], [e2b], scale=1.0 / 16)
            if GLA_STOP < 6.3:
                continue
            for hh in range(2):
                op_stt(P, qdT[:, hh, :, :], qTs[:, :, csl], C.cF[:, CF_HM + hh:CF_HM + hh + 1],
                       e1[:, :].rearrange("p (c t) -> p c t", c=2), ALU.mult, ALU.mult, [qTsb, e1b, C.cFb], [qdTb])
                op_stt(P, kdT[:, hh, :, :], kTs[:, :, csl], C.cF[:, CF_HM + hh:CF_HM + hh + 1],
                       e2[:, :].rearrange("p (c t) -> p c t", c=2), ALU.mult, ALU.mult, [kTsb, e2b, C.cFb], [kdTb])
            if GLA_STOP < 6.4:
                continue
            op_copy(P, "dve", dec[:, :], e1[:, :].rearrange("p (c t) -> p c t", c=2)[:, :, 127], [e1b], [decb])
            if GLA_STOP < 7:
                continue
            for c in range(2):
                op_mm(P, ps[4][:, c * 128:(c + 1) * 128], C.cF[:, CF_REV:CF_REV + 128], Lt[:, c * 128:(c + 1) * 128], True, True,
                      [Ltb, C.cFb], [psb[4]])
            if GLA_STOP < 7.1:
                continue
            op_act(P, e3[:, :], ps[4][:, 0:256], AF.Exp, [psb[4]], [e3b], scale=-1.0 / 16)
            if GLA_STOP < 7.2:
                continue
            op_tt(P, "pool", kte[:, :], ktok[:, :], e3[:, :], ALU.mult, [ktokb, e3b], [kteb])
            if GLA_STOP < 8:
                continue
            for h in range(4):
                op_mm(P, ps[5][:, h * 128:(h + 1) * 128], kdT[:, h % 2, h // 2, :], qdT[:, h % 2, h // 2, :],
                      True, True, [kdTb, qdTb], [psb[5]])
            op_tt(P, "dve", AT[:, :], ps[5][:, :], C.cF[:, CF_MASK4:CF_MASK4 + 512], ALU.mult, [psb[5], C.cFb], [ATb])
            if GLA_STOP < 9:
                continue
            for h in range(4):
                po_ = (h % 2) * 64
                first = (tt == 0)
                op_mm(P, ps[1][:, h * 128:(h + 1) * 128], AT[:, h * 128:(h + 1) * 128], vtok[:, h * 128:(h + 1) * 128],
                      True, first, [ATb, vtokb], [psb[1]])
                if not first:
                    op_mm(P, ps[1][:, h * 128:(h + 1) * 128], qdT[:, h % 2, h // 2, :], Sbf[:, h // 2, :],
                          False, True, [qdTb, Sbfb[h // 2]], [psb[1]])
            for c in range(2):
                op_mm(P, ps[4][:, 256:512], kte[:, c * 128:(c + 1) * 128], vtok[:, c * 256:(c + 1) * 256], True, True,
                      [kteb, vtokb], [psb[4]])
                for hh in range(2):
                    pp = slice(hh * 64, hh * 64 + 64)
                    op_stt(P, St[pp, c, :], St[pp, c, :], dec[pp, c:c + 1], ps[4][pp, 256 + hh * 128:256 + (hh + 1) * 128],
                           ALU.mult, ALU.add, [Stb[c], decb, psb[4]], [Stb[c]])
                op_copy(P, "act", Sbf[:, c, :], St[:, c, :], [Stb[c]], [Sbfb[c]])
            if GLA_STOP < 10:
                continue
            op_act(P, osq[:, :], ps[1][:, :], AF.Square, [psb[1]], [osqb])
            P.op("dve", lambda e: e.tensor_reduce(out=sm[:, 0:4], in_=osq[:, :].rearrange("p (h v) -> p h v", h=4),
                                                  axis=AX.X, op=ALU.add), reads=[osqb], writes=[smb[0]])
            op_ts(P, "dve", sm[:, 4:8], sm[:, 0:4], 1.0 / 128, EPS, ALU.mult, ALU.add, [smb[0]], [smb[1]])
            op_act(P, sm[:, 8:12], sm[:, 4:8], AF.Sqrt, [smb[1]], [smb[2]])
            P.op("dve", lambda e: e.reciprocal(out=sm[:, 12:16], in_=sm[:, 8:12]), reads=[smb[2]], writes=[smb[3]])
            for h in range(4):
                op_stt(P, t1[:, h * 128:(h + 1) * 128], ps[1][:, h * 128:(h + 1) * 128], sm[:, 12 + h:13 + h], gnB[:, :],
                       ALU.mult, ALU.mult, [psb[1], smb[3], gwb], [t1b])
            op_tt(P, "dve", og[:, :], t1[:, :], sgo[:, :], ALU.mult, [t1b, sgob], [ogb])
            k = tt % 2
            for h in range(4):
                P.op("pe", lambda e, h=h, k=k: e.transpose(out=C.psT[k][:, h * 128:(h + 1) * 128],
                                                           in_=og[:, h * 128:(h + 1) * 128], identity=C.ident[:, :]),
                     reads=[ogb, C.identb], writes=[C.psTb[k]])
            op_copy(P, "act", ogT[k][:, :, :], C.psT[k][:, 0:512].rearrange("p (c t) -> p c t", c=4), [C.psTb[k]], [ogTb[k]])
            P.dma("sp", omix_v[:, 0:4, ttl], ogT[k][:, :, :], reads=[ogTb[k]], writes=[omb])

    P.barrier(keep=True)
    P.sb_off = mark
    wf = P.sb([128, 8, 1544], BF16, "wf")
    wfb = P.buf("wf")
    P.dma("pool", wf[:, :, :], w_in_v[:, :, 1552:3096], reads=[], writes=[wfb])
    vaug = P.sb([128, 32, 8, 65], BF16, "vaug")
    vaugb = P.buf("vaug")
    P.op("pool", lambda e: e.memset(vaug[:, :, :, 64:65], 1.0), reads=[], writes=[vaugb])
    for tt in (range(32) if "v" in PARTS else []):
        r = tt % 3
        for dc in range(8):
            op_mm(P, ps[r][:, :], hT[:, dc, tt * 128:(tt + 1) * 128], wf[:, dc, 1024:1536], dc == 0, dc == 7,
                  [wfb, hTb[tt]], [psb[r]])
        op_copy(P, "act" if tt % 2 else "dve", vaug[:, tt, :, 0:64], ps[r][:, :].rearrange("p (h d) -> p h d", h=8),
                [psb[r]], [vaugb])
    mark2 = P.sb_off
    fb = P.sb([8, 2], F32, "fb")
    fbb = P.buf("fb")
    P.dma("sp", fb[:, 0:1], fox_b.rearrange("(h o) -> h o", o=1), reads=[], writes=[fbb])
    op_ts(P, "dve", fb[:, 1:2], fb[:, 0:1], -1.0, None, ALU.mult, None, [fbb], [fbb])
    zer = P.sb([8, 1024], F32, "zer")
    zerb = P.buf("zer")
    P.op("pool", lambda e: e.memset(zer[:, :], 0.0), reads=[], writes=[zerb])
    lT = P.sb([8, 1024], F32, "lT")
    lTb = P.buf("lT")
    fe = P.sb([8, 512], F32, "fe")
    feb = P.buf("fe")
    Fp = P.sb([8, 1024], F32, "Fp")
    Fpb = P.buf("Fp")
    r1 = P.sb([8, 1024], F32, "r1")
    r2 = P.sb([8, 1024], F32, "r2")
    r1b, r2b = P.buf("r1"), P.buf("r2")
    spl = P.sb([8, 6, 1024], BF16, "spl")
    splb = P.buf("spl")
    carry = P.sb([8, 1], F32, "carry")
    carryb = P.buf("carry")
    P.op("dve", lambda e: e.memset(carry[:, :], 0.0), reads=[], writes=[carryb])
    for pc in (range(4) if "s" in PARTS else []):
        for s2 in range(2):
            sb_ = pc * 2 + s2
            for dc in range(8):
                op_mm(P, ps[3][0:8, :], wf[:, dc, 1536:1544], hT[:, dc, sb_ * 512:(sb_ + 1) * 512], dc == 0, dc == 7,
                      [wfb] + hTb[sb_ * 4:sb_ * 4 + 4], [psb[3]])
            op_act(P, fe[:, :], ps[3][0:8, :], AF.Exp, [psb[3], fbb], [feb], scale=-1.0, bias=fb[:, 1:2])
            op_act(P, lT[:, s2 * 512:(s2 + 1) * 512], fe[:, :], AF.Ln, [feb], [lTb], bias=1.0)
        P.op("dve", lambda e: e.tensor_tensor_scan(out=Fp[:, :], data0=lT[:, :], data1=zer[:, :], initial=carry[:, 0:1],
                                                   op0=ALU.add, op1=ALU.add), reads=[lTb, zerb, carryb], writes=[Fpb])
        op_copy(P, "dve", carry[:, :], Fp[:, 1023:1024], [Fpb], [carryb])
        op_copy(P, "dve", spl[:, 3, :], Fp[:, :], [Fpb], [splb])
        op_tt(P, "dve", r1[:, :], Fp[:, :], spl[:, 3, :], ALU.subtract, [Fpb, splb], [r1b])
        op_copy(P, "dve", spl[:, 4, :], r1[:, :], [r1b], [splb])
        op_tt(P, "dve", r2[:, :], r1[:, :], spl[:, 4, :], ALU.subtract, [r1b, splb], [r2b])
        op_copy(P, "dve", spl[:, 5, :], r2[:, :], [r2b], [splb])
        op_ts(P, "dve", spl[:, 0:3, :], spl[:, 3:6, :], -1.0, None, ALU.mult, None, [splb], [splb])
        for side in range(2):
            P.dma("sp", fsplit_dram[side, :, :, pc * 1024:(pc + 1) * 1024], spl[:, side * 3:side * 3 + 3, :],
                  reads=[splb], writes=[fsb])
    P.barrier(keep=True)
    P.sb_off = mark2
    qa = [P.sb([70, S], BF16, "qa") for _ in range(2)]
    ka = [P.sb([70, S], BF16, "ka") for _ in range(2)]
    qab, kab = P.bufs_n(2, "qa"), P.bufs_n(2, "ka")
    pts = [P.sb([128, 512], BF16, "pt") for _ in range(3)]
    ptb = P.bufs_n(3, "pt")
    fin = alloc_fin(P)
    C.rot = C.rotp = C.rotf = 0
    for h in (range(8) if "h" in PARTS else []):
        k = h % 2
        P.op("pool", lambda e, k=k: e.memset(qa[k][64:70, :], 1.0), reads=[], writes=[qab[k]])
        P.op("pool", lambda e, k=k: e.memset(ka[k][64:70, :], 1.0), reads=[], writes=[kab[k]])
        P.dma("sp", qa[k][64:67, :], fsplit_dram[0, h, :, :], reads=[fsb], writes=[qab[k]])
        P.dma("sp", ka[k][67:70, :], fsplit_dram[1, h, :, :], reads=[fsb], writes=[kab[k]])
        for sb_ in range(8):
            tsl = slice(sb_ * 512, (sb_ + 1) * 512)
            hb = hTb[sb_ * 4:sb_ * 4 + 4]
            r = C.rot % 3
            C.rot += 1
            for dc in range(8):
                op_mm(P, ps[r][0:64, :], wf[:, dc, h * 64:(h + 1) * 64], hT[:, dc, tsl], dc == 0, dc == 7, [wfb] + hb, [psb[r]])
            op_act(P, qa[k][0:64, tsl], ps[r][0:64, :], AF.Copy, [psb[r]], [qab[k]], scale=0.125)
            r = C.rot % 3
            C.rot += 1
            for dc in range(8):
                op_mm(P, ps[r][0:64, :], wf[:, dc, 512 + h * 64:512 + (h + 1) * 64], hT[:, dc, tsl], dc == 0, dc == 7,
                      [wfb] + hb, [psb[r]])
            op_copy(P, "dve", ka[k][0:64, tsl], ps[r][0:64, :], [psb[r]], [kab[k]])

        def out_cb(qs, po, pob, h=h):
            attn_finalize(P, C, po, pob, omixT_dram[512 + h * 64:512 + (h + 1) * 64, qs * 512:(qs + 1) * 512], omb, fin)
        attention_head(P, C, ka[k][0:70, :], kab[k], qa[k][0:70, :], qab[k], lambda kc, h=h: vaug[:, kc, h, :], vaugb,
                       65, ptb, pts, out_cb, fox=True)

    P.barrier(keep=True)
    P.sb_off = mark
    if "p" in PARTS:
        mixer_out_proj(P, C, src, xs, xbuf, omixT_dram, omb, w_out)
    P.barrier()


def mixer_out_proj(P, C, src, xs, xbuf, omixT_dram, omb, w_out):
    ps, psb = C.ps, C.psb
    om = P.sb([128, 8, S], BF16, "om")
    omsb = P.bufs_n(8, "om")
    omix_v = omixT_dram.rearrange("(c p) t -> p c t", p=128)
    for c in range(8):
        P.dma("sp", om[:, c, :], omix_v[:, c, :], reads=[omb], writes=[omsb[c]])
    wo = P.sb([128, 8, D], BF16, "wo")
    wob = P.buf("wo")
    P.dma("pool", wo[:, :, :], w_out.rearrange("(c p) n -> p c n", p=128), reads=[], writes=[wob])
    xt = P.sb([128, 4, D], F32, "xt2")
    xtb = P.bufs_n(4, "xt2")
    for tt in range(32):
        i = tt % 4
        P.dma("sp", xt[:, i, :], src[tt * 128:(tt + 1) * 128, :], reads=[xbuf], writes=[xtb[i]])
        for n in range(2):
            r = (tt * 2 + n) % 4
            for c in range(8):
                op_mm(P, ps[r][:, :], om[:, c, tt * 128:(tt + 1) * 128], wo[:, c, n * 512:(n + 1) * 512], c == 0, c == 7,
                      [omsb[c], wob], [psb[r]])
            op_tt(P, "dve", xt[:, i, n * 512:(n + 1) * 512], ps[r][:, :], xt[:, i, n * 512:(n + 1) * 512], ALU.add,
                  [psb[r], xtb[i]], [xtb[i]])
        P.dma("sp", xs[tt * 128:(tt + 1) * 128, :], xt[:, i, :], reads=[xtb[i]], writes=[xbuf])


NBIS = 13


def odd_phase(P, C, src, xs, xbuf, g_dram, w_in, kv_g, w_uk, w_uv, w_out, t5, omixT_dram, nsT_dram, qT_dram):
    alloc_common(P, C)
    load_consts(P, C)
    C = copy.copy(C)
    ps, psb = C.ps, C.psb
    omb = P.buf("omixT_dram")
    nsb_d = P.buf("nsT_dram")
    qTb_d = P.buf("qT_dram")
    w_in_v = w_in.rearrange("(dc p) n -> p dc n", p=128)
    cT = P.sb([128, 2, S], BF16, "cT")
    cTb = P.bufs_n(32, "cT")
    mark0 = P.sb_off
    kidx2 = P.sb([128, S], BF16, "kidx2")
    kidx2b = P.buf("kidx2")
    qidx = P.sb([128, 4, S], BF16, "qidx")
    qidxb = P.bufs_n(8, "qidx")
    wq = P.sb([128, 32, 8], F32, "wq")
    wqb = P.bufs_n(32, "wq")
    pre_hT = P.sb_off
    hT, hTb = build_hT_full(P, C, src, xbuf, g_dram)
    mark1 = C.mark
    if ODD_STOP < 1:
        P.barrier()
        return
    wA = P.sb([128, 8, 840], BF16, "wA")
    wAb = P.buf("wA")
    P.dma("pool", wA[:, :, :], w_in_v[:, :, 1024:1864], reads=[], writes=[wAb])
    wQ = P.sb([128, 8, 1024], BF16, "wQ")
    wQb = P.buf("wQ")
    P.dma("pool", wQ[:, :, :], w_in_v[:, :, 0:1024], reads=[], writes=[wQb])
    wk2 = P.sb([128, 8, 128], BF16, "wk2")
    wk2b = P.buf("wk2")
    P.dma("pool", wk2[:, :, 0:64], w_in_v[:, :, 1792:1856], reads=[], writes=[wk2b])
    P.dma("pool", wk2[:, :, 64:128], w_in_v[:, :, 1792:1856], reads=[], writes=[wk2b])
    gkv = P.sb([128, 256], F32, "gkv")
    gkvb = P.buf("gkv")
    P.dma("sp", gkv[:, :], kv_g.partition_broadcast(128), reads=[], writes=[gkvb])
    cjunk = P.sb([128, 256], BF16, "cjunk")
    cjunkb = P.buf("cjunk")
    cs = P.sb([128, 8], F32, "cs")
    csb = P.bufs_n(4, "cs")
    ctok = [P.sb([128, 256], BF16, "ctok") for _ in range(2)]
    ctokb = P.bufs_n(2, "ctok")
    qst = [P.sb([128, 512], BF16, "qst") for _ in range(2)]
    qstb = P.bufs_n(2, "qst")
    rr = 0
    for sb_ in range(8):
        tsl = slice(sb_ * 512, (sb_ + 1) * 512)
        hb = hTb[sb_ * 4:sb_ * 4 + 4]
        r = rr % 4
        rr += 1
        for dc in range(8):
            op_mm(P, ps[r][:, :], wk2[:, dc, :], hT[:, dc, tsl], dc == 0, dc == 7, [wk2b] + hb, [psb[r]])
        op_copy(P, "act", kidx2[:, tsl], ps[r][:, :], [psb[r]], [kidx2b])
        for p4 in range(4):
            r = rr % 4
            rr += 1
            for dc in range(8):
                op_mm(P, ps[r][:, :], wA[:, dc, 256 + p4 * 128:256 + (p4 + 1) * 128], hT[:, dc, tsl], dc == 0, dc == 7,
                      [wAb] + hb, [psb[r]])
            op_copy(P, "dve" if p4 % 2 else "act", qidx[:, p4, tsl], ps[r][:, :], [psb[r]], [qidxb[sb_]])
        for pr in range(8):
            r = rr % 4
            rr += 1
            for dc in range(8):
                op_mm(P, ps[r][:, :], wQ[:, dc, pr * 128:(pr + 1) * 128], hT[:, dc, tsl], dc == 0, dc == 7, [wQb] + hb, [psb[r]])
            k = pr % 2
            op_act(P, qst[k][:, :], ps[r][:, :], AF.Copy, [psb[r]], [qstb[k]], scale=0.125)
            P.dma("sp", qT_dram[pr * 128:(pr + 1) * 128, tsl], qst[k][:, :], reads=[qstb[k]], writes=[qTb_d])
        for ch in range(4):
            tt = sb_ * 4 + ch
            ttl = slice(tt * 128, (tt + 1) * 128)
            k = tt % 2
            for dc in range(8):
                op_mm(P, ps[4][:, 0:256], hT[:, dc, ttl], wA[:, dc, 0:256], dc == 0, dc == 7, [wAb, hTb[tt]], [psb[4]])
            for dc in range(8):
                op_mm(P, ps[5][:, 0:128], hT[:, dc, ttl], wA[:, dc, 712:840], dc == 0, dc == 7, [wAb, hTb[tt]], [psb[5]])
            op_copy(P, "dve", wq[:, tt, :], ps[5][:, 120:128], [psb[5]], [wqb[tt]])
            op_act(P, cjunk[:, :], ps[4][:, 0:256], AF.Square, [psb[4]], [cjunkb, csb[0]], accum_out=cs[:, 0:1])
            op_ts(P, "dve", cs[:, 1:2], cs[:, 0:1], 1.0 / 256, EPS, ALU.mult, ALU.add, [csb[0]], [csb[1]])
            op_act(P, cs[:, 2:3], cs[:, 1:2], AF.Sqrt, [csb[1]], [csb[2]])
            P.op("dve", lambda e: e.reciprocal(out=cs[:, 3:4], in_=cs[:, 2:3]), reads=[csb[2]], writes=[csb[3]])
            op_stt(P, ctok[k][:, :], ps[4][:, 0:256], cs[:, 3:4], gkv[:, :], ALU.mult, ALU.mult, [psb[4], csb[3], gkvb], [ctokb[k]])
            for lc in range(2):
                P.op("pe", lambda e, lc=lc, k=k: e.transpose(out=C.psT[k][:, lc * 128:(lc + 1) * 128],
                                                             in_=ctok[k][:, lc * 128:(lc + 1) * 128], identity=C.ident[:, :]),
                     reads=[ctokb[k], C.identb], writes=[C.psTb[k]])
            op_copy(P, "act", cT[:, :, ttl], C.psT[k][:, 0:256].rearrange("p (c t) -> p c t", c=2), [C.psTb[k]], [cTb[tt]])
    if ODD_STOP < 2:
        P.barrier()
        return
    P.barrier(keep=True)
    P.sb_off = pre_hT
    sc = [P.sb([128, S], F32, "sc") for _ in range(2)]
    scb = P.bufs_n(2, "sc")
    cjk = P.sb([128, S], BF16, "cjk")
    cjkb = P.buf("cjk")
    nsel = P.sb([128, S], BF16, "nsel")
    nselb = P.buf("nsel")
    nst = P.sb([128, 32, 128], BF16, "nst")
    nstb = P.buf("nst")
    rl = [P.sb([128, 512], F32, "rl") for _ in range(3)]
    rlb = P.bufs_n(3, "rl")
    bs = P.sb([128, 8 + NBIS + 1], F32, "bs")
    bsb = P.bufs_n(8, "bs")
    stepb = P.buf("step")
    qm = P.sb([128, 8, 128], BF16, "qm")
    qmb = P.buf("qm")
    zt = P.sb([128, 128], BF16, "zt")
    ztb = P.buf("zt")
    P.op("pool", lambda e: e.memset(zt[:, :], 0.0), reads=[], writes=[ztb])
    P.dma("sp", nsT_dram[0, :, 0:128], C.negtri[:, :], reads=[C.negtrib], writes=[nsb_d])
    P.dma("sp", nsT_dram[0, :, 128:256], zt[:, :], reads=[ztb], writes=[nsb_d])
    P.dma("sp", nsT_dram[1, :, 128:256], C.negtri[:, :], reads=[C.negtrib], writes=[nsb_d])
    rr = 0
    bs2 = [bs, P.sb([128, 8 + NBIS + 1], F32, "bs1")]
    bsb2 = [bsb, P.bufs_n(8, "bs1")]
    stepb2 = [stepb, P.buf("step1")]
    cjk2 = [cjk, P.sb([128, S], BF16, "cjk1")]
    cjkb2 = [cjkb, P.buf("cjk1")]

    def score_tile(qc):
        nonlocal rr
        L = (qc + 1) * 128
        qsl = slice(qc * 128, (qc + 1) * 128)
        k = qc % 2
        s_, s_b = sc[k], scb[k]
        bs_, bsb_, stepb_ = bs2[k], bsb2[k], stepb2[k]
        nck = (L + 511) // 512
        for h in range(8):
            op_ts(P, "pool", qm[:, h, :], qidx[:, h // 2, qsl], C.cF[:, CF_HM + h % 2:CF_HM + h % 2 + 1], None, ALU.mult, None,
                  [qidxb[qc // 4], C.cFb], [qmb])
        for ck in range(nck):
            n = min(512, L - ck * 512)
            for h in range(8):
                r = rr % 4
                rr += 1
                op_mm(P, ps[r][:, 0:n], qm[:, h, :], kidx2[:, ck * 512:ck * 512 + n], True, True, [qmb, kidx2b], [psb[r]])
                r3 = rr % 3
                op_act(P, rl[r3][:, 0:n], ps[r][:, 0:n], AF.Relu, [psb[r]], [rlb[r3]])
                if h == 0:
                    op_ts(P, "dve", s_[:, ck * 512:ck * 512 + n], rl[r3][:, 0:n], wq[:, qc, 0:1], None, ALU.mult, None,
                          [rlb[r3], wqb[qc]], [s_b])
                else:
                    op_stt(P, s_[:, ck * 512:ck * 512 + n], rl[r3][:, 0:n], wq[:, qc, h:h + 1], s_[:, ck * 512:ck * 512 + n],
                           ALU.mult, ALU.add, [rlb[r3], wqb[qc], s_b], [s_b])
        P.op("pool", lambda e: e.affine_select(out=s_[:, L - 128:L], in_=s_[:, L - 128:L], pattern=[[-1, 128]],
                                                compare_op=ALU.is_ge, fill=-1.0e30, base=0, channel_multiplier=1),
             reads=[s_b], writes=[s_b])
        P.op("dve", lambda e: e.tensor_reduce(out=bs_[:, 0:1], in_=s_[:, 0:L], axis=AX.X, op=ALU.max),
             reads=[s_b], writes=[bsb_[0]])
        P.op("dve", lambda e: e.tensor_reduce(out=bs_[:, 1:2], in_=s_[:, 0:L - 128], axis=AX.X, op=ALU.min),
             reads=[s_b], writes=[bsb_[1]])
        op_tt(P, "dve", bs_[:, 2:3], bs_[:, 0:1], bs_[:, 1:2], ALU.subtract, [bsb_[0], bsb_[1]], [bsb_[2]])
        op_ts(P, "dve", bs_[:, 8:8 + NBIS + 1], C.cF[:, CF_POW:CF_POW + NBIS + 1], bs_[:, 2:3], None, ALU.mult, None,
              [bsb_[2], C.cFb], [stepb_])
        op_stt(P, bs_[:, 3:4], bs_[:, 2:3], 0.5, bs_[:, 1:2], ALU.mult, ALU.add, [bsb_[2], bsb_[1]], [bsb_[3]])

    def bis_iter(qc, it):
        L = (qc + 1) * 128
        k = qc % 2
        s_, s_b = sc[k], scb[k]
        bs_, bsb_, stepb_ = bs2[k], bsb2[k], stepb2[k]
        cj, cjb = cjk2[k], cjkb2[k]
        P.op("dve", lambda e: e.tensor_scalar(out=cj[:, 0:L], in0=s_[:, 0:L], scalar1=bs_[:, 3:4], scalar2=None,
                                              op0=ALU.is_ge, op1=ALU.add, accum_out=bs_[:, 4:5]),
             reads=[s_b, bsb_[3]], writes=[cjb, bsb_[4]])
        op_ts(P, "dve", bs_[:, 5:6], bs_[:, 4:5], 255.5, 0.5, ALU.is_ge, ALU.subtract, [bsb_[4]], [bsb_[5]])
        op_stt(P, bs_[:, 3:4], bs_[:, 8 + it:9 + it], bs_[:, 5:6], bs_[:, 3:4], ALU.mult, ALU.add, [stepb_, bsb_[5], bsb_[3]], [bsb_[3]])

    def finish_tile(qc):
        L = (qc + 1) * 128
        qsl = slice(qc * 128, (qc + 1) * 128)
        k = qc % 2
        s_, s_b = sc[k], scb[k]
        bs_, bsb_, stepb_ = bs2[k], bsb2[k], stepb2[k]
        op_tt(P, "dve", bs_[:, 6:7], bs_[:, 3:4], bs_[:, 8 + NBIS:9 + NBIS], ALU.subtract, [bsb_[3], stepb_], [bsb_[6]])
        P.op("pool", lambda e: e.tensor_scalar(out=nsel[:, 0:L], in0=s_[:, 0:L], scalar1=bs_[:, 6:7], scalar2=-30000.0,
                                               op0=ALU.is_lt, op1=ALU.mult),
             reads=[s_b, bsb_[6]], writes=[nselb])
        for g in range((qc + 8) // 8):
            kk = g % 2
            nb_ = min(8, qc + 1 - g * 8)
            for j in range(nb_):
                kc = g * 8 + j
                P.op("pe", lambda e, kc=kc, j=j, kk=kk: e.transpose(out=C.psT[kk][:, j * 128:(j + 1) * 128],
                                                                    in_=nsel[:, kc * 128:(kc + 1) * 128], identity=C.ident[:, :]),
                     reads=[nselb, C.identb], writes=[C.psTb[kk]])
            op_copy(P, "act", nst[:, g * 8:g * 8 + nb_, :], C.psT[kk][:, 0:nb_ * 128].rearrange("p (c t) -> p c t", c=nb_),
                    [C.psTb[kk]], [nstb])
        P.dma("sp", nsT_dram[0:qc + 1, :, qsl].rearrange("k s t -> s k t"), nst[:, 0:qc + 1, :], reads=[nstb], writes=[nsb_d])

    for q0 in (range(2, 32, 2) if "2" in OPARTS else []):
        pair = (q0, q0 + 1)
        for qc in pair:
            score_tile(qc)
        for it in range(NBIS):
            for qc in pair:
                bis_iter(qc, it)
        for qc in pair:
            finish_tile(qc)
    if ODD_STOP < 3:
        P.barrier()
        return
    P.barrier(keep=True)
    P.sb_off = mark0
    wukf = P.sb([128, 8, 256], F32, "wukf")
    wukfb = P.buf("wukf")
    P.dma("sp", wukf[:, :, :], w_uk.rearrange("(pr hh) d l -> (hh d) pr l", hh=2), reads=[], writes=[wukfb])
    wukT = P.sb([128, 2, 8, 128], BF16, "wukT")
    wukTb = P.buf("wukT")
    for pr in (range(8) if "a" in OPARTS else []):
        for lc in range(2):
            r = (pr * 2 + lc) % 4
            P.op("pe", lambda e, pr=pr, lc=lc, r=r: e.transpose(out=ps[r][:, 0:128], in_=wukf[:, pr, lc * 128:(lc + 1) * 128],
                                                                identity=C.identf[:, :]),
                 reads=[wukfb, C.identb], writes=[psb[r]])
            op_copy(P, "act" if lc else "dve", wukT[:, lc, pr, :], ps[r][:, 0:128], [psb[r]], [wukTb])
    wuv = P.sb([128, 2, 16, 64], BF16, "wuv")
    wuvb = P.buf("wuv")
    for lc in range(2):
        P.dma("pool", wuv[:, lc, :, :], w_uv[:, lc * 128:(lc + 1) * 128, :].rearrange("h p d -> p h d"), reads=[], writes=[wuvb])
    if ODD_STOP < 4:
        P.barrier()
        return
    tab = P.sb([32, 16], F32, "tab")
    tab31 = P.sb([32, 16], F32, "tab31")
    tabb = P.buf("tab")
    P.dma("sp", tab[:, :], t5, reads=[], writes=[tabb])
    P.dma("sp", tab31[:, :], t5[31, :].partition_broadcast(32), reads=[], writes=[tabb])
    tab2 = P.sb([32, 16], F32, "tab2")
    tab2b = P.buf("tab2")
    op_tt(P, "dve", tab2[:, :], tab[:, :], tab31[:, :], ALU.subtract, [tabb], [tab2b])
    ohr = P.sb([32, 512], F32, "ohr")
    ohrb = P.buf("ohr")
    P.dma("sp", ohr[:, :], C.ohrev_dram, reads=[], writes=[ohrb])
    biasT = P.sb([128, 16, 256], BF16, "biasT")
    biasTb = P.buf("biasT")
    tab2p = P.sb([32, 128], F32, "tab2p")
    tab2pb = P.buf("tab2p")
    P.op("dve", lambda e: e.memset(tab2p[:, :], 0.0), reads=[], writes=[tab2pb])
    op_copy(P, "dve", tab2p[:, 0:16], tab2[:, :], [tab2b], [tab2pb])
    gi = 0
    for typ in range(2):
        for g in range(32):
            r = gi % 4
            gi += 1
            for tl in range(4):
                t = g * 4 + tl
                st = (383 - t) if typ == 0 else (255 - t)
                op_mm(P, ps[r][:, tl * 128:(tl + 1) * 128], ohr[:, st:st + 128], tab2p[:, :], True, True, [ohrb, tab2pb], [psb[r]])
            op_copy(P, "act" if g % 2 else "dve", biasT[:, :, typ * 128 + g * 4:typ * 128 + (g + 1) * 4],
                    ps[r][:, :].rearrange("p (t c) -> p t c", t=4)[:, :, 0:16].rearrange("p t h -> p h t"), [psb[r]], [biasTb])
    if ODD_STOP < 5:
        P.barrier()
        return
    mark3 = P.sb_off
    qT = [P.sb([128, S], BF16, "qT") for _ in range(1)]
    kT = [P.sb([128, S], BF16, "kT") for _ in range(1)]
    qTb, kTb = P.bufs_n(1, "qT"), P.bufs_n(1, "kT")
    qTm = [P.sb([128, S], BF16, "qTm") for _ in range(2)]
    qTmb = P.bufs_n(2, "qTm")
    vaug = P.sb([128, 32, 2, 65], BF16, "vaug2")
    vaugb = P.buf("vaug2")
    nstl = [P.sb([128, 32, 512], BF16, "nstl") for _ in range(2)]
    nstlb = P.bufs_n(2, "nstl")
    pts = [P.sb([128, 512], BF16, "pt") for _ in range(3)]
    ptb = P.bufs_n(3, "pt")
    fin = alloc_fin(P)
    P.op("pool", lambda e: e.memset(vaug[:, :, :, 64:65], 1.0), reads=[], writes=[vaugb])
    nload = 0
    for pr in (range(8) if "c" in OPARTS else []):
        P.dma("sp", qT[0][:, :], qT_dram[pr * 128:(pr + 1) * 128, :], reads=[qTb_d], writes=[qTb[0]])
        for hh in range(2):
            op_ts(P, "pool", qTm[hh][:, :], qT[0][:, :], C.cF[:, CF_HM + hh:CF_HM + hh + 1], None, ALU.mult, None,
                  [qTb[0], C.cFb], [qTmb[hh]])
        for sb_ in range(8):
            tsl = slice(sb_ * 512, (sb_ + 1) * 512)
            r = C.rot % 3
            C.rot += 1
            for lc in range(2):
                op_mm(P, ps[r][:, :], wukT[:, lc, pr, :], cT[:, lc, tsl], lc == 0, lc == 1, [wukTb] + cTb[sb_ * 4:sb_ * 4 + 4], [psb[r]])
            op_copy(P, "dve", kT[0][:, tsl], ps[r][:, :], [psb[r]], [kTb[0]])
        for tt in range(32):
            r = C.rot % 3
            C.rot += 1
            for lc in range(2):
                op_mm(P, ps[r][:, 0:128], cT[:, lc, tt * 128:(tt + 1) * 128],
                      wuv[:, lc, 2 * pr:2 * pr + 2, :], lc == 0, lc == 1, [wuvb, cTb[tt]], [psb[r]])
            op_copy(P, "act", vaug[:, tt, :, 0:64], ps[r][:, 0:128].rearrange("p (h d) -> p h d", h=2), [psb[r]], [vaugb])
        for qs in range(8):
            nk = 4 * qs + 4
            kb = nload % 2
            nload += 1
            if qs > 0:
                P.dma("sp", nstl[kb][:, 0:4 * qs, :], nsT_dram[0:4 * qs, :, qs * 512:(qs + 1) * 512].rearrange("k s t -> s k t"),
                      reads=[nsb_d], writes=[nstlb[kb]])
            for kc in range(4 * qs, nk):
                c0 = (kc - 4 * qs) * 128
                P.dma("sp", nstl[kb][:, kc, c0:512], nsT_dram[kc, :, kc * 128:(qs + 1) * 512], reads=[nsb_d], writes=[nstlb[kb]])
            for hh in range(2):
                h = 2 * pr + hh
                pp = slice(hh * 64, hh * 64 + 64)

                def out_cb(qs_, po, pob, h=h):
                    attn_finalize(P, C, po, pob, omixT_dram[h * 64:(h + 1) * 64, qs_ * 512:(qs_ + 1) * 512], omb, fin)
                attention_head(P, C, kT[0][:, :], kTb[0], qTm[hh][:, :], qTmb[hh], lambda kc, hh=hh: vaug[:, kc, hh, :], vaugb,
                               65, ptb, pts, out_cb,
                               negsel=lambda qs_, kb=kb: ((lambda kc: nstl[kb][:, kc, :]), nstlb[kb]),
                               nearb=(biasT[:, h, :], biasTb), qss=[qs])
    if ODD_STOP < 6:
        P.barrier()
        return
    P.barrier(keep=True)
    P.sb_off = SB_BASE + 16 * 1024
    if "4" in OPARTS:
        mixer_out_proj(P, C, src, xs, xbuf, omixT_dram, omb, w_out)
    P.barrier()


PHASES_ALL = ("f00", "even", "f01", "f10", "odd", "f11")


def build(phases=PHASES_ALL):
    nc = bass.Bass("TRN2", target_bir_lowering=False)
    dt = lambda name, shape, kind="ExternalInput", dtype=F32: nc.dram_tensor(name, list(shape), dtype, kind=kind).ap()
    x = dt("x", [S, D])
    norm_g = dt("norm_g", [2, 3, D])
    ffn_w_in = dt("ffn_w_in", [2, 2, D, 2 * DFF])
    ffn_w_out = dt("ffn_w_out", [2, 2, DFF, D])
    final_norm_g = dt("final_norm_g", [D])
    even_w_in = dt("even_w_in", [D, 3096])
    gla_w_gate = dt("gla_w_gate", [16, 256])
    gla_b_gate = dt("gla_b_gate", [256])
    gla_norm_g = dt("gla_norm_g", [128])
    fox_b_f = dt("fox_b_f", [8])
    even_w_out = dt("even_w_out", [D, D])
    odd_w_in = dt("odd_w_in", [D, 1864])
    mla_kv_norm_g = dt("mla_kv_norm_g", [256])
    mla_w_uk = dt("mla_w_uk", [16, 64, 256])
    mla_w_uv = dt("mla_w_uv", [16, 256, 64])
    odd_w_out = dt("odd_w_out", [D, D])
    t5_table = dt("t5_table", [32, 16])
    ident_bf = dt("ident_bf", [128, 128], dtype=BF16)
    ident_f = dt("ident_f", [128, 128])
    cF = dt("cF", [128, CF_N])
    negtri = dt("negtri", [128, 128], dtype=BF16)
    ohvec = dt("ohvec", [32, 512])
    out = dt("out", [S, D], kind="ExternalOutput")
    xs = dt("xs", [S, D], kind="Internal")
    omixT = dt("omixT", [D, S], kind="Internal", dtype=BF16)
    fsplit = dt("fsplit", [2, 8, 3, S], kind="Internal", dtype=BF16)
    nsT = dt("nsT", [32, 128, S], kind="Internal", dtype=BF16)
    qTd = dt("qTd", [D, S], kind="Internal", dtype=BF16)

    P = Prog(nc)
    C = Ctx()
    C.ident_dram = ident_bf
    C.identf_dram = ident_f
    C.cF_dram = cF
    C.negtri_dram = negtri
    C.ohrev_dram = ohvec
    C.ps = [nc.alloc_psum_tensor("ps%d" % i, [128, 512], F32) for i in range(6)]
    C.psb = [P.buf("ps%d" % i) for i in range(6)]
    C.psT = [nc.alloc_psum_tensor("psT%d" % i, [128, 1024], BF16) for i in range(2)]
    C.rot = C.rotp = C.rotf = C.roto = 0

    xbuf = P.buf("xs")
    C.outbuf = P.buf("out")
    cur = x
    for ph in phases:
        if ph.startswith("f") and ph != "fin":
            l, k = int(ph[1]), int(ph[2])
            last = (ph == phases[-1])
            ffn_phase_wrap(P, C, cur, xs, xbuf, norm_g[l, 2 * k, :], ffn_w_in[l, k], ffn_w_out[l, k],
                           final_norm_g if last else None, out)
            cur = xs
        elif ph == "even":
            even_phase(P, C, cur, xs, xbuf, norm_g[0, 1, :], even_w_in, gla_w_gate, gla_b_gate, gla_norm_g, fox_b_f,
                       even_w_out, omixT, fsplit)
            cur = xs
        elif ph == "odd":
            odd_phase(P, C, cur, xs, xbuf, norm_g[1, 1, :], odd_w_in, mla_kv_norm_g, mla_w_uk, mla_w_uv, odd_w_out,
                      t5_table, omixT, nsT, qTd)
            cur = xs
        elif ph == "fin":
            final_phase(P, C, xs, xbuf, final_norm_g, out)
        else:
            raise NotImplementedError(ph)
    P.emit()
    return nc


def final_phase(P, C, xs, xbuf, fg, out):
    alloc_common(P, C)
    C.fgB = P.sb([128, D], F32, "fgB")
    C.fgBb = P.buf("fgB")
    P.dma("sp", C.fgB[:, :], fg.partition_broadcast(128), reads=[], writes=[C.fgBb])
    C = copy.copy(C)
    xt = P.sb([128, 4, D], F32, "xt")
    xtb = P.bufs_n(4, "xt")
    for tt in range(32):
        i = tt % 4
        P.dma("sp", xt[:, i, :], xs[tt * 128:(tt + 1) * 128, :], reads=[xbuf], writes=[xtb[i]])
        final_norm_tile(P, C, xt, xtb, i, tt, fg, out)
    P.barrier()


def ffn_phase_wrap(P, C, src, dst, xbuf, g, w_in, w_out, fg, out):
    if fg is not None:
        C.fgB = P.sb([128, D], F32, "fgB")
        C.fgBb = P.buf("fgB")
        P.dma("sp", C.fgB[:, :], fg.partition_broadcast(128), reads=[], writes=[C.fgBb])
    ffn_phase(P, C, src, dst, xbuf, g, w_in, w_out, fg, out)


_CACHE = {}


def kernel(**inputs):
    import ml_dtypes
    phases = inputs.pop("_phases", PHASES_ALL)
    key = tuple(phases)
    if key not in _CACHE:
        _CACHE[key] = build(phases)
    nc = _CACHE[key]
    x = np.ascontiguousarray(inputs["x"], dtype=np.float32)
    shared = {
        "norm_g": np.ascontiguousarray(inputs["norm_g"], dtype=np.float32),
        "ffn_w_in": np.ascontiguousarray(inputs["ffn_w_in"], dtype=np.float32),
        "ffn_w_out": np.ascontiguousarray(inputs["ffn_w_out"], dtype=np.float32),
        "final_norm_g": np.ascontiguousarray(inputs["final_norm_g"], dtype=np.float32),
        "ident_bf": np.eye(128, dtype=np.float32).astype(ml_dtypes.bfloat16),
        "ident_f": np.eye(128, dtype=np.float32),
        "even_w_in": np.ascontiguousarray(inputs["even_w_in"][0], dtype=np.float32),
        "gla_w_gate": np.ascontiguousarray(inputs["gla_w_gate"][0], dtype=np.float32),
        "gla_b_gate": np.ascontiguousarray(inputs["gla_b_gate"][0], dtype=np.float32),
        "gla_norm_g": np.ascontiguousarray(inputs["gla_norm_g"][0], dtype=np.float32),
        "fox_b_f": np.ascontiguousarray(inputs["fox_b_f"][0], dtype=np.float32),
        "even_w_out": np.ascontiguousarray(inputs["even_w_out"][0], dtype=np.float32),
        "odd_w_in": np.ascontiguousarray(inputs["odd_w_in"][0], dtype=np.float32),
        "mla_kv_norm_g": np.ascontiguousarray(inputs["mla_kv_norm_g"][0], dtype=np.float32),
        "mla_w_uk": np.ascontiguousarray(inputs["mla_w_uk"][0], dtype=np.float32),
        "mla_w_uv": np.ascontiguousarray(inputs["mla_w_uv"][0], dtype=np.float32),
        "odd_w_out": np.ascontiguousarray(inputs["odd_w_out"][0], dtype=np.float32),
        "t5_table": np.ascontiguousarray(inputs["t5_table"], dtype=np.float32),
    }
    cF_, negtri_ = host_consts()
    shared["cF"] = cF_
    shared["negtri"] = negtri_
    shared["ohvec"] = np.ascontiguousarray(host_ohvec()[:, ::-1])
    in_maps = []
    for c in range(8):
        m = dict(shared)
        m["x"] = x[c]
        in_maps.append(m)
    res = run_bass_kernel_spmd(nc, in_maps, core_ids=list(range(8)))
    return np.stack([np.asarray(r["out"], dtype=np.float32) for r in res.results], axis=0)
```
